# Optimizing a Trainium2 kernel written in Bass

```python
import math
import jax, jax.numpy as jnp
from jax import lax
import numpy as np

D_MODEL = 1024
BATCH = 8
SEQ = 2048
DEPTH = 2

EPS = 1e-6
POOL_WINDOWS = (2, 4, 8, 16)
N_POOL_GROUPS = len(POOL_WINDOWS)
POOL_GROUP_DIM = D_MODEL // N_POOL_GROUPS
HEAD_DIM = 64
HEADS_PER_GROUP = D_MODEL // HEAD_DIM
DILATED_PATTERNS = ((128, 1), (512, 4), (2048, 16))
N_ATT_GROUPS = len(DILATED_PATTERNS)
N_HEADS_TOTAL = N_ATT_GROUPS * HEADS_PER_GROUP
Q_WIDTH = N_HEADS_TOTAL * HEAD_DIM
KV_WIDTH = 2 * Q_WIDTH
MERGED_WIDTH = HEADS_PER_GROUP * HEAD_DIM
QBLK = 128
NEG_INF = -1e30
N_BUCKETS = 32
MAX_EXACT = N_BUCKETS // 2
MAX_DISTANCE = 2048
D_FF_DENSE = ((8 * D_MODEL // 3 + 255) // 256) * 256
N_EXPERTS = 8
TOP_K = 2
D_FF_EXPERT = 7 * D_MODEL // 2
MOE_BLOCK = 256
N_A_LAYERS = DEPTH // 2
N_B_LAYERS = DEPTH - N_A_LAYERS
N_DENSE = (DEPTH + 1) // 2
N_MOE = DEPTH // 2

kernel_name = "yoco_pool_dilated_moe_hybrid"


def rmsnorm(x, g):
    xf = x.astype(jnp.float32)
    y = xf * lax.rsqrt(jnp.mean(xf * xf, axis=-1, keepdims=True) + EPS)
    return (y * g.astype(jnp.float32)).astype(x.dtype)


def pool_mixer(xn, proj, scale):
    B, S, D = xn.shape
    xg = xn.astype(jnp.float32).reshape(B, S, N_POOL_GROUPS, POOL_GROUP_DIM)
    c = jnp.pad(jnp.cumsum(xg, axis=1), ((0, 0), (1, 0), (0, 0), (0, 0)))
    hi = jnp.arange(1, S + 1, dtype=jnp.int32)
    win = jnp.array(POOL_WINDOWS, dtype=jnp.int32)[:, None]
    lo = jnp.maximum(hi[None, :] - win, 0)
    c_lo = c[:, lo.T, jnp.arange(N_POOL_GROUPS)[None, :], :]
    cnt = (hi[None, :] - lo).astype(jnp.float32).T
    pooled = (c[:, 1:] - c_lo) / cnt[None, :, :, None] - xg
    out = jnp.einsum('bsgc,gcd->bsgd', pooled, proj.astype(jnp.float32))
    out = out.reshape(B, S, D) * scale.astype(jnp.float32)
    return out.astype(xn.dtype)


def t5_bucket(n):
    nf = jnp.maximum(n, 1).astype(jnp.float32)
    large = MAX_EXACT + (jnp.log(nf / MAX_EXACT) / math.log(MAX_DISTANCE / MAX_EXACT)
                         * (N_BUCKETS - MAX_EXACT)).astype(jnp.int32)
    large = jnp.minimum(large, N_BUCKETS - 1)
    return jnp.where(n < MAX_EXACT, n, large)


def dilated_group_attention(q, k, v, bias_table, window, dilation):
    B, S, H, Dh = q.shape
    W = window // dilation
    assert W <= QBLK
    L = S // dilation
    n = -(-L // QBLK)
    Lp = n * QBLK

    def to_sub(t, left):
        t = t.reshape(B, L, dilation, H, Dh).transpose(0, 2, 1, 3, 4)
        return jnp.pad(t, ((0, 0), (0, 0), (left, Lp - L), (0, 0), (0, 0)))

    def key_blocks(t):
        t = to_sub(t, QBLK).reshape(B, dilation, n + 1, QBLK, H, Dh)
        return jnp.concatenate([t[:, :, :-1], t[:, :, 1:]], axis=3)

    qs = to_sub(q, 0).reshape(B, dilation, n, QBLK, H, Dh)
    kb = key_blocks(k)
    vb = key_blocks(v)

    a = jnp.arange(QBLK, dtype=jnp.int32)[:, None]
    c = jnp.arange(2 * QBLK, dtype=jnp.int32)[None, :]
    m = a + QBLK - c
    band = (m >= 0) & (m <= W)
    kpos = jnp.arange(n, dtype=jnp.int32)[:, None, None] * QBLK - QBLK + c[None]
    valid = band[None] & (kpos >= 0) & (kpos < L)
    bias = bias_table[t5_bucket(jnp.maximum(m, 0) * dilation)]
    bias = bias.transpose(2, 0, 1).astype(jnp.float32)

    logits = jnp.einsum('brnqhd,brnkhd->brnhqk', qs, kb,
                        preferred_element_type=jnp.float32) * (HEAD_DIM ** -0.5)
    logits = jnp.where(valid[None, None, :, None], logits + bias[None, None, None], NEG_INF)
    mx = jnp.max(logits, axis=-1, keepdims=True)
    p = jnp.exp(logits - mx)
    s = jnp.sum(p, axis=-1)
    o = jnp.einsum('brnhqk,brnkhd->brnqhd', p, vb.astype(jnp.float32))
    s_t = s.transpose(0, 1, 2, 4, 3)
    o = o / s_t[..., None]
    lse = (mx[..., 0] + jnp.log(s)).transpose(0, 1, 2, 4, 3)

    o = o.reshape(B, dilation, Lp, H, Dh)[:, :, :L].transpose(0, 2, 1, 3, 4).reshape(B, S, H, Dh)
    lse = lse.reshape(B, dilation, Lp, H)[:, :, :L].transpose(0, 2, 1, 3).reshape(B, S, H)
    return o, lse


def dilated_attention_layer(hn, w_q, w_o, k_sh, v_sh, rel_bias):
    B, S, _ = hn.shape
    q = (hn @ w_q).reshape(B, S, N_ATT_GROUPS, HEADS_PER_GROUP, HEAD_DIM)
    outs, lses = [], []
    for g, (window, dilation) in enumerate(DILATED_PATTERNS):
        o, lse = dilated_group_attention(
            q[:, :, g], k_sh[:, :, g], v_sh[:, :, g],
            rel_bias[:, g * HEADS_PER_GROUP:(g + 1) * HEADS_PER_GROUP], window, dilation)
        outs.append(o)
        lses.append(lse)
    alpha = jax.nn.softmax(jnp.stack(lses, axis=0), axis=0)
    o = jnp.einsum('gbsh,gbshd->bshd', alpha, jnp.stack(outs, axis=0))
    return o.reshape(B, S, MERGED_WIDTH).astype(hn.dtype) @ w_o


def dense_swiglu(xn, w_gate, w_up, w_down):
    return (jax.nn.silu(xn @ w_gate) * (xn @ w_up)) @ w_down


def moe_swiglu(xn, w_router, w_gate, w_up, w_down):
    B, S, D = xn.shape
    T = B * S
    xf = xn.reshape(T, D)
    logits = (xf @ w_router).astype(jnp.float32)
    top_val, top_idx = lax.top_k(logits, TOP_K)
    gates = jax.nn.softmax(top_val, axis=-1)
    e_flat = top_idx.reshape(-1).astype(jnp.int32)
    tok = jnp.repeat(jnp.arange(T, dtype=jnp.int32), TOP_K)
    g_flat = gates.reshape(-1)
    order = jnp.argsort(e_flat)
    e_s, tok_s, g_s = e_flat[order], tok[order], g_flat[order]

    n_assign = T * TOP_K
    n_pad = n_assign + N_EXPERTS * MOE_BLOCK
    n_blocks = n_pad // MOE_BLOCK
    counts = jnp.zeros((N_EXPERTS,), jnp.int32).at[e_flat].add(1)
    padded = ((counts + MOE_BLOCK - 1) // MOE_BLOCK) * MOE_BLOCK
    ends_pad = jnp.cumsum(padded)
    starts_pad = ends_pad - padded
    starts_raw = jnp.cumsum(counts) - counts
    rank = jnp.arange(n_assign, dtype=jnp.int32) - starts_raw[e_s]
    dest = starts_pad[e_s] + rank
    block_start = jnp.arange(n_blocks, dtype=jnp.int32) * MOE_BLOCK
    blk_e = jnp.minimum(jnp.searchsorted(ends_pad, block_start, side='right'),
                        N_EXPERTS - 1).astype(jnp.int32)
    xs = jnp.zeros((n_pad, D), xn.dtype).at[dest].set(xf[tok_s])

    def expert_block(args):
        xb, e = args
        h = jax.nn.silu(xb @ w_gate[e]) * (xb @ w_up[e])
        return h @ w_down[e]

    ys = lax.map(expert_block, (xs.reshape(n_blocks, MOE_BLOCK, D), blk_e)).reshape(n_pad, D)
    y = jnp.zeros((T, D), jnp.float32).at[tok_s].add(
        g_s[:, None] * ys[dest].astype(jnp.float32))
    return y.reshape(B, S, D).astype(xn.dtype)


def setup_inputs(seed: int = 0) -> dict:
    key = jax.random.key(seed)
    ks = jax.random.split(key, 20)

    def nrm(k, shape, fan_in):
        return jax.random.normal(k, shape, jnp.float32) * (fan_in ** -0.5)

    def gain(k, shape):
        return 1.0 + 0.05 * jax.random.normal(k, shape, jnp.float32)

    return {
        "x": jax.random.normal(ks[0], (BATCH, SEQ, D_MODEL), jnp.float32),
        "a_norm": gain(ks[1], (N_A_LAYERS, D_MODEL)),
        "a_proj": nrm(ks[2], (N_A_LAYERS, N_POOL_GROUPS, POOL_GROUP_DIM, POOL_GROUP_DIM), POOL_GROUP_DIM),
        "a_scale": gain(ks[3], (N_A_LAYERS, D_MODEL)),
        "kv_norm": gain(ks[4], (D_MODEL,)),
        "w_kv": nrm(ks[5], (D_MODEL, KV_WIDTH), D_MODEL),
        "b_norm": gain(ks[6], (N_B_LAYERS, D_MODEL)),
        "w_q": nrm(ks[7], (N_B_LAYERS, D_MODEL, Q_WIDTH), D_MODEL),
        "w_o": nrm(ks[8], (N_B_LAYERS, MERGED_WIDTH, D_MODEL), MERGED_WIDTH),
        "rel_bias": 0.5 * jax.random.normal(ks[9], (N_BUCKETS, N_HEADS_TOTAL), jnp.float32),
        "ffn_norm": gain(ks[10], (DEPTH, D_MODEL)),
        "dense_w_gate": nrm(ks[11], (N_DENSE, D_MODEL, D_FF_DENSE), D_MODEL),
        "dense_w_up": nrm(ks[12], (N_DENSE, D_MODEL, D_FF_DENSE), D_MODEL),
        "dense_w_down": nrm(ks[13], (N_DENSE, D_FF_DENSE, D_MODEL), D_FF_DENSE),
        "moe_router": nrm(ks[14], (N_MOE, D_MODEL, N_EXPERTS), D_MODEL),
        "moe_w_gate": nrm(ks[15], (N_MOE, N_EXPERTS, D_MODEL, D_FF_EXPERT), D_MODEL),
        "moe_w_up": nrm(ks[16], (N_MOE, N_EXPERTS, D_MODEL, D_FF_EXPERT), D_MODEL),
        "moe_w_down": nrm(ks[17], (N_MOE, N_EXPERTS, D_FF_EXPERT, D_MODEL), D_FF_EXPERT),
        "final_norm": gain(ks[18], (D_MODEL,)),
    }


def reference(x, a_norm, a_proj, a_scale, kv_norm, w_kv, b_norm, w_q, w_o, rel_bias,
              ffn_norm, dense_w_gate, dense_w_up, dense_w_down,
              moe_router, moe_w_gate, moe_w_up, moe_w_down, final_norm):
    B, S, _ = x.shape
    h = x
    k_sh = v_sh = None
    for l in range(DEPTH):
        if l < N_A_LAYERS:
            h = h + pool_mixer(rmsnorm(h, a_norm[l]), a_proj[l], a_scale[l])
        else:
            if l == N_A_LAYERS:
                kv = (rmsnorm(h, kv_norm) @ w_kv).reshape(
                    B, S, 2, N_ATT_GROUPS, HEADS_PER_GROUP, HEAD_DIM)
                k_sh, v_sh = kv[:, :, 0], kv[:, :, 1]
            j = l - N_A_LAYERS
            h = h + dilated_attention_layer(rmsnorm(h, b_norm[j]), w_q[j], w_o[j],
                                            k_sh, v_sh, rel_bias)
        hn = rmsnorm(h, ffn_norm[l])
        if l % 2 == 0:
            i = l // 2
            h = h + dense_swiglu(hn, dense_w_gate[i], dense_w_up[i], dense_w_down[i])
        else:
            i = l // 2
            h = h + moe_swiglu(hn, moe_router[i], moe_w_gate[i], moe_w_up[i], moe_w_down[i])
    return rmsnorm(h, final_norm)
```

```python
import contextlib
import math
import numpy as np
import concourse.bass as bass
import concourse.mybir as mybir
from concourse.bass_utils import run_bass_kernel_spmd

F32 = mybir.dt.float32
BF16 = mybir.dt.bfloat16
AF = mybir.ActivationFunctionType
ALU = mybir.AluOpType
AX = mybir.AxisListType

NCORES = 8
S = 2048
D = 1024
NT = S // 128
KD = D // 128
EPS = 1e-6
FF_DENSE = 2816
FF_EXP = 3584
NEXP = 8
POOL_W = (2, 4, 8, 16)
DIL = ((128, 1), (512, 4), (2048, 16))
NEG = -30000.0
CAP = 640
NCT = CAP // 128


class Buf:
    __slots__ = ("w", "r", "name", "nfull")

    def __init__(self, name=""):
        self.w = []
        self.r = []
        self.nfull = 0
        self.name = name


class Prog:
    def __init__(self, nc, st):
        self.nc = nc
        self.st = st
        self.eng = {"pe": nc.tensor, "act": nc.scalar, "dve": nc.vector,
                    "pool": nc.gpsimd, "sp": nc.sync}
        self.sems = {}
        self.cnt = {}
        for e in ("pe", "act", "dve", "pool"):
            self.sems[e] = st.enter_context(nc.semaphore("s_" + e))
            self.cnt[e] = 0
        self.seen = {e: {} for e in self.eng}
        self.out_toks = []

    def dsem(self, name):
        self.sems[name] = self.st.enter_context(self.nc.semaphore(name))
        self.cnt[name] = 0
        return name

    def _wait(self, e, toks):
        need = {}
        for t in toks:
            key, val, src = t
            if src == "pe" and e == "pe":
                continue
            if need.get(key, 0) < val:
                need[key] = val
        seen = self.seen[e]
        for key, val in need.items():
            if seen.get(key, 0) >= val:
                continue
            seen[key] = val
            self.eng[e].wait_ge(self.sems[key], val)

    def _deps(self, r, w, pw):
        toks = []
        for b in r:
            toks += b.w
        for b in w:
            toks += b.w
            toks += b.r
        for b in pw:
            toks += b.w[:b.nfull]
            toks += b.r
        return toks

    def _upd(self, tok, r, w, pw):
        for b in r:
            b.r.append(tok)
        for b in w:
            b.w = [tok]
            b.nfull = 1
            b.r = []
        for b in pw:
            b.w.append(tok)

    def op(self, e, fn, r=(), w=(), pw=()):
        self._wait(e, self._deps(r, w, pw))
        ins = fn(self.eng[e])
        self.cnt[e] += 1
        ins.then_inc(self.sems[e], 1)
        tok = (e, self.cnt[e], e)
        self._upd(tok, r, w, pw)
        return tok

    def dma(self, q, dkey, out, in_, r=(), w=(), pw=(), **kw):
        self._wait(q, self._deps(r, w, pw))
        ins = self.eng[q].dma_start(out=out, in_=in_, **kw)
        self.cnt[dkey] += 16
        ins.then_inc(self.sems[dkey], 16)
        tok = (dkey, self.cnt[dkey], "dma")
        self._upd(tok, r, w, pw)
        return tok

    def barrier(self):
        toks = [(k, v, "bar") for k, v in self.cnt.items() if v > 0]
        for e in self.eng:
            self._wait(e, toks)

    def finish(self):
        self._wait("sp", self.out_toks)


def _t5_bucket(n):
    n = np.asarray(n, np.int64)
    nf = np.maximum(n, 1).astype(np.float32)
    large = 16 + (np.log(nf / np.float32(16)) / np.float32(math.log(2048 / 16))
                  * np.float32(16)).astype(np.int32)
    large = np.minimum(large, 31)
    return np.where(n < 16, n, large)


def _host_consts():
    ident = np.eye(128, dtype=np.float32)
    band = np.zeros((4, 3, 128, 128), np.float32)
    tp = np.arange(128)[:, None]
    t = np.arange(128)[None, :]
    for g, w in enumerate(POOL_W):
        inwin = (tp <= t) & (tp > t - w)
        cnt0 = np.minimum(t + 1, w).astype(np.float32)
        band[g, 0] = inwin / cnt0 - (tp == t)
        band[g, 1] = inwin / np.float32(w) - (tp == t)
        band[g, 2] = ((tp + 0 > t + 128 - w)) / np.float32(w)
    oh = np.zeros((3, 64, 512), np.float32)
    for g, (_, dil) in enumerate(DIL):
        for idx in range(255):
            m = idx - 127
            if m >= 0:
                oh[g, int(_t5_bucket(m * dil)), idx] = 1.0
            else:
                oh[g, 32, idx] = NEG
            m2 = idx + 1
            if m2 <= 128:
                oh[g, int(_t5_bucket(m2 * dil)), 256 + idx] = 1.0
            else:
                oh[g, 32, 256 + idx] = NEG
        oh[g, 32, 255] = NEG
        oh[g, 32, 511] = NEG
    iota = np.tile(np.arange(CAP, dtype=np.float32)[None], (128, 1))
    tri = (np.arange(128)[:, None] < np.arange(128)[None, :]).astype(np.float32)
    return {"c_ident": ident, "c_band": band, "c_oh": oh, "c_iota": iota, "c_tri": tri}


class Ctx:
    pass


def _alloc_common(nc, st, P):
    C = Ctx()
    C.nc, C.st, C.P = nc, st, P
    C.h = st.enter_context(nc.sbuf_tensor("h", [128, NT, D], F32))
    C.hB = [Buf("h%d" % i) for i in range(NT)]
    C.ps = [st.enter_context(nc.psum_tensor("ps%d" % i, [128, 512], F32)) for i in range(8)]
    C.psB = [Buf("ps%d" % i) for i in range(8)]
    C.ident = st.enter_context(nc.sbuf_tensor("ident", [128, 128], BF16))
    C.identB = Buf("ident")
    C.ss = st.enter_context(nc.sbuf_tensor("ss", [128, NT], F32))
    C.rstd = st.enter_context(nc.sbuf_tensor("rstd", [128, NT], F32))
    C.ssB = Buf("ss")
    C.rstdB = Buf("rstd")
    C.junk = st.enter_context(nc.sbuf_tensor("junk", [128, D], BF16))
    C.junkB = Buf("junk")
    C.d_const = None
    C.const_bufs = []
    C.d_io = P.dsem("d_io")
    return C


def _const_begin(C, name):
    C.d_const = C.P.dsem(name)
    C.const_bufs = []


def _const_fence(C):
    best = None
    for b in C.const_bufs:
        for t in b.w:
            if best is None or t[1] > best[1]:
                best = t
    for b in C.const_bufs:
        b.w = [best]
        b.nfull = 1
    C.const_bufs = []


def _load_ident(C, dram):
    C.P.dma("pool", C.d_const, C.ident[:], dram["c_ident"], w=[C.identB])
    C.const_bufs.append(C.identB)


def _load_gain_cols(C, name, src_ap):
    t = C.st.enter_context(C.nc.sbuf_tensor(name, [128, KD], F32))
    b = Buf(name)
    C.P.dma("sp", C.d_const, t[:], src_ap.rearrange("(k p) -> p k", p=128), w=[b],
            allow_slow_non_contiguous=True)
    C.const_bufs.append(b)
    return t, b


def _load_bcast_row(C, st, name, src_ap):
    t = st.enter_context(C.nc.sbuf_tensor(name, [128, D], F32))
    b = Buf(name)
    C.P.dma("sp", C.d_const, t[:], src_ap.unsqueeze(0).to_broadcast([128, D]), w=[b])
    C.const_bufs.append(b)
    return t, b


def _load_h(C, src):
    v = src.rearrange("(n p) d -> p n d", p=128)
    for i in range(0, NT, 4):
        C.P.dma("sp", C.d_io, C.h[:, i:i + 4, :], v[:, i:i + 4, :], w=C.hB[i:i + 4])
    last = C.hB[NT - 1].w
    for b in C.hB:
        b.w = list(last)
        b.nfull = len(b.w)


def _store_h(C, dst):
    v = dst.rearrange("(n p) d -> p n d", p=128)
    for i in range(0, NT, 4):
        tok = C.P.dma("sp", C.d_io, v[:, i:i + 4, :], C.h[:, i:i + 4, :], r=C.hB[i:i + 4])
        C.P.out_toks.append(tok)


def _rms_stats(C):
    P = C.P
    P.op("dve", lambda e: e.memset(C.ss[:], 0.0), w=[C.ssB])
    for i in range(NT):
        P.op("act", lambda e, i=i: e.activation(out=C.junk[:], in_=C.h[:, i, :], func=AF.Square,
                                                accum_out=C.ss[:, i:i + 1]),
             r=[C.hB[i]], w=[C.junkB], pw=[C.ssB])
    P.op("act", lambda e: e.activation(out=C.rstd[:], in_=C.ss[:], func=AF.Sqrt,
                                       scale=1.0 / D, bias=EPS),
         r=[C.ssB], w=[C.rstdB])
    P.op("dve", lambda e: e.reciprocal(out=C.rstd[:], in_=C.rstd[:]), r=[], w=[C.rstdB])


def _normalize_tm(C, xh, xhB):
    P = C.P
    for i in range(NT):
        if i % 2 == 0:
            P.op("dve", lambda e, i=i: e.tensor_scalar(out=xh[:, i, :], in0=C.h[:, i, :],
                                                       scalar1=C.rstd[:, i:i + 1], scalar2=None,
                                                       op0=ALU.mult),
                 r=[C.hB[i], C.rstdB], w=[xhB[i]])
        else:
            P.op("act", lambda e, i=i: e.activation(out=xh[:, i, :], in_=C.h[:, i, :], func=AF.Copy,
                                                    scale=C.rstd[:, i:i + 1]),
                 r=[C.hB[i], C.rstdB], w=[xhB[i]])


def _transpose_gain(C, xh, xhB, XT, XTB, gcol, gB, col0=0):
    P = C.P
    for i in range(NT):
        bank = i % 2
        psb = C.ps[bank].bitcast(BF16)

        def tr(e, i=i, psb=psb):
            ins = None
            for k in range(KD):
                ins = e.transpose(psb[:, k * 128:(k + 1) * 128], xh[:, i, k * 128:(k + 1) * 128],
                                  C.ident[:])
            return ins
        P.op("pe", tr, r=[xhB[i], C.identB], w=[C.psB[bank]])
        eng = "dve" if i % 2 == 0 else "act"
        src = psb[:, :].rearrange("p (k t) -> p k t", k=KD)
        dst = XT[:, :, col0 + i * 128: col0 + (i + 1) * 128]
        if eng == "dve":
            P.op("dve", lambda e, src=src, dst=dst: e.tensor_tensor(
                out=dst, in0=src, in1=gcol[:, :].unsqueeze(2).to_broadcast([128, KD, 128]),
                op=ALU.mult), r=[C.psB[bank], gB], pw=[XTB[i // 4]])
        else:
            def ev(e, src=src, dst=dst):
                ins = None
                for k in range(KD):
                    ins = e.activation(out=dst[:, k, :], in_=src[:, k, :], func=AF.Copy,
                                       scale=gcol[:, k:k + 1])
                return ins
            P.op("act", ev, r=[C.psB[bank], gB], pw=[XTB[i // 4]])


def _ffn(C, st, XT, XTB, wg, wu, wd, F, tag, gate=None, gateB=None, gate_e=0,
         chunks=None, tgt=None, tgtB=None):
    P, nc = C.P, C.nc
    W = C.ffn_w
    if chunks is None:
        chunks = [(c * 512, 512) for c in range(4)]
    if tgt is None:
        tgt, tgtB = C.h, C.hB
    groups = []
    f0 = 0
    while f0 < F:
        fw = min(W.gw, F - f0)
        groups.append((f0, fw))
        f0 += fw
    wgv = wg.rearrange("(k p) f -> p k f", p=128)
    wuv = wu.rearrange("(k p) f -> p k f", p=128)
    for (f0, fw) in groups:
        nfc = fw // 128
        s = W.slot % 2
        W.slot += 1
        gsb, usb, dsb = W.g[s], W.u[s], W.d[s]
        bG, bU, bD = W.gB[s], W.uB[s], W.dB[s]
        P.dma("pool", W.sem[s], gsb[:, :, 0:fw], wgv[:, :, f0:f0 + fw], w=[bG])
        P.dma("pool", W.sem[s], usb[:, :, 0:fw], wuv[:, :, f0:f0 + fw], w=[bU])
        P.dma("pool", W.sem[s], dsb[:, 0:nfc, :],
              wd[f0:f0 + fw, :].rearrange("(c p) d -> p c d", p=128), w=[bD])
        allw = bG.w + bU.w + bD.w
        for b_ in (bG, bU, bD):
            b_.w = list(allw)
            b_.nfull = len(allw)
        for c, (t0_, tw_) in enumerate(chunks):
            tsl = slice(t0_, t0_ + tw_)
            for fc in range(nfc):
                pg = W.rot % 2
                W.rot += 1
                gi, ui = 2 * pg, 2 * pg + 1

                def mm(e, src, bank, fc=fc, tsl=tsl, tw_=tw_):
                    ins = None
                    for k in range(KD):
                        ins = e.matmul(C.ps[bank][:, 0:tw_], src[:, k, fc * 128:(fc + 1) * 128],
                                       XT[:, k, tsl], start=(k == 0), stop=(k == KD - 1))
                    return ins
                P.op("pe", lambda e, gi=gi, mm=mm: mm(e, gsb, gi), r=[bG, XTB[c]], w=[C.psB[gi]])
                P.op("pe", lambda e, ui=ui, mm=mm: mm(e, usb, ui), r=[bU, XTB[c]], w=[C.psB[ui]])
                sgt = W.sg[pg]
                P.op("act", lambda e, gi=gi, sgt=sgt, tw_=tw_: e.activation(out=sgt[:, 0:tw_], in_=C.ps[gi][:, 0:tw_],
                                                                            func=AF.Silu),
                     r=[C.psB[gi]], w=[W.sgB[pg]])
                P.op("dve", lambda e, ui=ui, sgt=sgt, fc=fc, tw_=tw_: e.tensor_tensor(
                    out=W.ht[:, fc, 0:tw_], in0=sgt[:, 0:tw_], in1=C.ps[ui][:, 0:tw_], op=ALU.mult),
                    r=[W.sgB[pg], C.psB[ui]], w=[W.htB[fc]])
            for half in range(2):
                for tt in range(tw_ // 128):
                    ti = t0_ // 128 + tt
                    yb = 4 + (W.yrot % 4)
                    W.yrot += 1

                    def dn(e, yb=yb, tt=tt, half=half):
                        ins = None
                        for fc in range(nfc):
                            ins = e.matmul(C.ps[yb][:, :], W.ht[:, fc, tt * 128:(tt + 1) * 128],
                                           dsb[:, fc, half * 512:(half + 1) * 512],
                                           start=(fc == 0), stop=(fc == nfc - 1))
                        return ins
                    P.op("pe", dn, r=[bD] + W.htB[0:nfc], w=[C.psB[yb]])
                    hs = tgt[:, ti, half * 512:(half + 1) * 512]
                    if gate is None:
                        P.op("dve", lambda e, yb=yb, hs=hs: e.tensor_tensor(
                            out=hs, in0=C.ps[yb][:, :], in1=hs, op=ALU.add),
                            r=[C.psB[yb]], w=[tgtB[ti]])
                    else:
                        gap = gate[:, ti, gate_e:gate_e + 1]
                        P.op("dve", lambda e, yb=yb, hs=hs, gap=gap: e.scalar_tensor_tensor(
                            out=hs, in0=C.ps[yb][:, :], scalar=gap, in1=hs,
                            op0=ALU.mult, op1=ALU.add),
                            r=[C.psB[yb], gateB], w=[tgtB[ti]])


def _alloc_ffn(C, st, tag="", gw=512):
    nc, P = C.nc, C.P
    W = Ctx()
    W.slot = 0
    W.gw = gw
    W.rot = 0
    W.yrot = 0
    W.g = [st.enter_context(nc.sbuf_tensor(tag + "wg%d" % s, [128, KD, gw], BF16)) for s in range(2)]
    W.u = [st.enter_context(nc.sbuf_tensor(tag + "wu%d" % s, [128, KD, gw], BF16)) for s in range(2)]
    W.d = [st.enter_context(nc.sbuf_tensor(tag + "wd%d" % s, [128, gw // 128, D], BF16)) for s in range(2)]
    W.gB = [Buf() for _ in range(2)]
    W.uB = [Buf() for _ in range(2)]
    W.dB = [Buf() for _ in range(2)]
    W.sem = [P.dsem("d_ffw%s%d" % (tag, s)) for s in range(2)]
    W.sg = [st.enter_context(nc.sbuf_tensor(tag + "sg%d" % s, [128, 512], BF16)) for s in range(2)]
    W.sgB = [Buf() for _ in range(2)]
    W.ht = st.enter_context(nc.sbuf_tensor(tag + "ht", [128, gw // 128, 512], BF16))
    W.htB = [Buf() for _ in range(4)]
    C.ffn_w = W


def phase_a(C, dram):
    nc, P = C.nc, C.P
    with contextlib.ExitStack() as st:
        _const_begin(C, "d_constA")
        gA, gAB = _load_gain_cols(C, "gA", dram["a_norm"])
        gF, gFB = _load_gain_cols(C, "gF0", dram["ffn_norm0"])
        asc, ascB = _load_bcast_row(C, st, "asc", dram["a_scale"])
        xh = st.enter_context(nc.sbuf_tensor("xh", [128, NT, D], BF16))
        xhB = [Buf() for _ in range(NT)]
        XT = st.enter_context(nc.sbuf_tensor("XT", [128, KD, S], BF16))
        XTB = [Buf() for _ in range(4)]
        band = st.enter_context(nc.sbuf_tensor("band", [128, 12, 128], BF16))
        bandB = Buf()
        P.dma("pool", C.d_const, band[:], dram["c_band"].rearrange("g j a b -> a (g j) b"), w=[bandB])
        C.const_bufs.append(bandB)
        pstage = st.enter_context(nc.sbuf_tensor("pstage", [128, 8, 256], F32))
        pw = st.enter_context(nc.sbuf_tensor("pw", [128, 8, 256], BF16))
        pstB, pwB = Buf(), Buf()
        P.dma("sp", C.d_const, pstage[:], dram["a_proj"].rearrange("g (j p) f -> p (g j) f", p=128),
              w=[pstB])
        C.const_bufs.append(pstB)
        _const_fence(C)
        for g in range(4):
            P.op("pool", lambda e, g=g: e.tensor_tensor(
                out=pw[:, 2 * g:2 * g + 2, :], in0=pstage[:, 2 * g:2 * g + 2, :],
                in1=asc[:, g * 256:(g + 1) * 256].unsqueeze(1).to_broadcast([128, 2, 256]),
                op=ALU.mult), r=[pstB, ascB], pw=[pwB])

        _rms_stats(C)
        _normalize_tm(C, xh, xhB)
        for i in range(NT):
            for kk in range(2):
                bank = (2 * i + kk) % 4

                def pm(e, i=i, kk=kk, bank=bank):
                    ins = None
                    for j in range(4):
                        k = kk * 4 + j
                        g = k // 2
                        o = C.ps[bank][:, j * 128:(j + 1) * 128]
                        if i == 0:
                            ins = e.matmul(o, xh[:, 0, k * 128:(k + 1) * 128], band[:, 3 * g + 0, :],
                                           start=True, stop=True, skip_group_check=True)
                        else:
                            e.matmul(o, xh[:, i - 1, k * 128:(k + 1) * 128], band[:, 3 * g + 2, :],
                                     start=True, stop=False, skip_group_check=True)
                            ins = e.matmul(o, xh[:, i, k * 128:(k + 1) * 128], band[:, 3 * g + 1, :],
                                           start=False, stop=True, skip_group_check=True)
                    return ins
                rd = [xhB[i], bandB] + ([xhB[i - 1]] if i > 0 else [])
                P.op("pe", pm, r=rd, w=[C.psB[bank]])
                src = C.ps[bank][:, :].rearrange("p (k t) -> p k t", k=4)
                dst = XT[:, kk * 4:(kk + 1) * 4, i * 128:(i + 1) * 128]
                P.op("dve", lambda e, src=src, dst=dst, kk=kk: e.tensor_tensor(
                    out=dst, in0=src,
                    in1=gA[:, kk * 4:(kk + 1) * 4].unsqueeze(2).to_broadcast([128, 4, 128]),
                    op=ALU.mult), r=[C.psB[bank], gAB], pw=[XTB[i // 4]])
        for i in range(NT):
            for half in range(2):
                bank = 4 + (2 * i + half) % 4

                def pj(e, i=i, half=half, bank=bank):
                    ins = None
                    for gg in range(2):
                        g = half * 2 + gg
                        for j in range(2):
                            ins = e.matmul(C.ps[bank][:, gg * 256:(gg + 1) * 256],
                                           XT[:, 2 * g + j, i * 128:(i + 1) * 128],
                                           pw[:, 2 * g + j, :], start=(j == 0), stop=(j == 1),
                                           skip_group_check=True)
                    return ins
                P.op("pe", pj, r=[XTB[i // 4], pwB], w=[C.psB[bank]])
                hs = C.h[:, i, half * 512:(half + 1) * 512]
                P.op("dve", lambda e, bank=bank, hs=hs: e.tensor_tensor(
                    out=hs, in0=C.ps[bank][:, :], in1=hs, op=ALU.add),
                    r=[C.psB[bank]], w=[C.hB[i]])

        if getattr(C, "dbg_stop", None) == "mixer":
            return
        _rms_stats(C)
        _normalize_tm(C, xh, xhB)
        _transpose_gain(C, xh, xhB, XT, XTB, gF, gFB)
        _alloc_ffn(C, st, "A")
        _ffn(C, st, XT, XTB, dram["dense_w_gate"], dram["dense_w_up"], dram["dense_w_down"],
             FF_DENSE, "dense")


_NBLK = (16, 4, 1)
_DILS = (1, 4, 16)
_ETAB = ((0, 0), (0, 1), (1, 0), (1, 1), (2, 0))


def _att_batches():
    out = []
    out += [(0, 0, [(0, n) for n in range(b, b + 4)]) for b in range(0, 16, 4)]
    prev = [(0, n) for n in range(1, 16)]
    out += [(0, 1, prev[b:b + 4]) for b in range(0, 15, 4)]
    for r in range(4):
        out.append((1, 0, [(r, n) for n in range(4)]))
        out.append((1, 1, [(r, n) for n in range(1, 4)]))
    out += [(2, 0, [(r, 0) for r in range(b, b + 4)]) for b in range(0, 16, 4)]
    return out


def phase_b(C, dram):
    nc, P = C.nc, C.P
    with contextlib.ExitStack() as st:
        _const_begin(C, "d_constB")
        gKV, gKVB = _load_gain_cols(C, "gKV", dram["kv_norm"])
        gQ, gQB = _load_gain_cols(C, "gQ", dram["b_norm"])
        relb = st.enter_context(nc.sbuf_tensor("relb", [64, 48], F32))
        relbB = Buf()
        P.dma("sp", C.d_const, relb[0:32, :], dram["rel_bias"], w=[relbB])
        C.const_bufs.append(relbB)
        oh = st.enter_context(nc.sbuf_tensor("oh", [64, 3, 512], BF16))
        ohB = Buf()
        P.dma("pool", C.d_const, oh[:], dram["c_oh"].rearrange("g b i -> b g i"), w=[ohB])
        C.const_bufs.append(ohB)
        _const_fence(C)
        P.op("dve", lambda e: e.memset(relb[32:64, :], 1.0), pw=[relbB])
        gone = st.enter_context(nc.sbuf_tensor("gone", [128, KD], F32))
        goneB = Buf()
        P.op("dve", lambda e: e.memset(gone[:], 1.0), w=[goneB])

        XT = st.enter_context(nc.sbuf_tensor("XTb", [128, KD, S], BF16))
        XTB = [Buf() for _ in range(4)]
        big = st.enter_context(nc.sbuf_tensor("bigb", [128, 21504], BF16))
        big_toks = []
        et = [st.enter_context(nc.sbuf_tensor("et%d" % s_, [128, 10, 128], BF16)) for s_ in range(2)]
        etB = [Buf() for _ in range(2)]
        d_et = [P.dsem("d_et%d" % s_) for s_ in range(2)]
        ws = [st.enter_context(nc.sbuf_tensor("wsb%d" % s_, [128, KD, 128], F32)) for s_ in range(2)]
        wsB = [Buf() for _ in range(2)]
        d_ws = [P.dsem("d_ws%d" % s_) for s_ in range(2)]
        wbf = [st.enter_context(nc.sbuf_tensor("wbf%d" % m, [128, KD, 128], BF16)) for m in range(9)]
        wbfB = [Buf() for _ in range(9)]
        pts = [st.enter_context(nc.sbuf_tensor("pt%d" % s_, [128, 512], BF16)) for s_ in range(3)]
        ptB = [Buf() for _ in range(3)]
        rc = st.enter_context(nc.sbuf_tensor("rc", [128, S], F32))
        rcB = Buf()
        rcB2 = [Buf() for _ in range(4)]
        OT = st.enter_context(nc.sbuf_tensor("OT", [128, S], BF16))
        otB = [Buf() for _ in range(4)]
        wo = [st.enter_context(nc.sbuf_tensor("wo%d" % s_, [128, D], BF16)) for s_ in range(2)]
        woB = [Buf() for _ in range(2)]
        d_wo = [P.dsem("d_wo%d" % s_) for s_ in range(2)]
        fv = [st.enter_context(nc.sbuf_tensor("fv%d" % s_, [128, 512], BF16)) for s_ in range(2)]
        fvB = [Buf() for _ in range(2)]
        d_fs = P.dsem("d_fs")
        fscr = nc.dram_tensor("fscr", [48, 128, 512], BF16)
        fscr_ap = fscr.ap()
        fscrB = Buf()

        rhi = st.enter_context(nc.sbuf_tensor("rhi", [64, 48], BF16))
        rlo = st.enter_context(nc.sbuf_tensor("rlo", [64, 48], BF16))
        rhiB, rloB = Buf(), Buf()
        P.op("dve", lambda e: e.tensor_copy(out=rhi[:], in_=relb[:]), r=[relbB], w=[rhiB])
        P.op("dve", lambda e: e.tensor_tensor(out=rlo[:], in0=relb[:], in1=rhi[:], op=ALU.subtract),
             r=[relbB, rhiB], w=[rloB])
        rbc_hi = big[0:64, 0:2048].rearrange("p (h c) -> p h c", c=128)
        rbc_lo = big[0:64, 2048:4096].rearrange("p (h c) -> p h c", c=128)
        relbcB = Buf()
        for g in range(3):
            P.op("dve", lambda e, g=g: e.tensor_copy(
                out=rbc_hi, in_=rhi[:, g * 16:(g + 1) * 16].unsqueeze(2).to_broadcast([64, 16, 128])),
                r=[rhiB], w=[relbcB])
            P.op("dve", lambda e, g=g: e.tensor_copy(
                out=rbc_lo, in_=rlo[:, g * 16:(g + 1) * 16].unsqueeze(2).to_broadcast([64, 16, 128])),
                r=[rloB], pw=[relbcB])
            for hh in range(16):
                gh = g * 16 + hh
                bank = 2 + gh % 2

                def eg(e, g=g, hh=hh, bank=bank):
                    e.matmul(C.ps[bank][:, :], rbc_hi[:, hh, :], oh[:, g, :], start=True, stop=False)
                    return e.matmul(C.ps[bank][:, :], rbc_lo[:, hh, :], oh[:, g, :], start=False, stop=True)
                P.op("pe", eg, r=[relbcB, ohB], w=[C.psB[bank]])
                P.op("act", lambda e, gh=gh, bank=bank: e.activation(
                    out=fv[gh % 2][:], in_=C.ps[bank][:, :], func=AF.Exp),
                    r=[C.psB[bank]], w=[fvB[gh % 2]])
                P.dma("sp", d_fs, fscr_ap[gh], fv[gh % 2][:], r=[fvB[gh % 2]], pw=[fscrB])
        big_toks += relbcB.w + relbcB.r

        xh = big[:, 0:NT * D].rearrange("p (i d) -> p i d", i=NT)
        xhB = [Buf() for _ in range(NT)]
        for b in xhB:
            b.r = list(big_toks)
        _rms_stats(C)
        _normalize_tm(C, xh, xhB)
        _transpose_gain(C, xh, xhB, XT, XTB, gone, goneB)
        for b in xhB:
            big_toks += b.w + b.r

        KT = big[:, 0:6144]
        QT = big[:, 6144:12288]
        Vall = big[:, 12288:21504].rearrange("p (g t c) -> p g t c", g=3, t=16)
        ktB = [Buf() for _ in range(3)]
        qtB = [Buf() for _ in range(3)]
        vB = [Buf() for _ in range(3)]
        for b in ktB + qtB + vB:
            b.r = list(big_toks)
        for g in range(3):
            P.op("dve", lambda e, g=g: e.memset(Vall[:, g, :, 64:128], 1.0), w=[vB[g]])

        wkv_v = dram["w_kv"].rearrange("(k p) f -> p k f", p=128)
        wq_v = dram["w_q"].rearrange("(k p) f -> p k f", p=128)
        state = {"ws": 0, "pr": 0, "sr": 0, "pt": 0}

        def load_pair_inputs(j):
            slot = j % 2
            for hl in range(2):
                hh = 2 * j + hl
                for ti, (g, half) in enumerate(_ETAB):
                    gh = g * 16 + hh
                    src = bass.AP(fscr, gh * 128 * 512 + half * 256 + 127, [[511, 128], [1, 128]])
                    P.dma("sp", d_et[slot], et[slot][:, hl * 5 + ti, :], src, r=[fscrB], pw=[etB[slot]])
            P.dma("pool", d_wo[slot], wo[slot][:], dram["w_o"][128 * j:128 * (j + 1), :], w=[woB[slot]])

        def load_pair_weights(j):
            for g in range(3):
                specs = ((wkv_v, g * 1024, gKV, gKVB), (wkv_v, 3072 + g * 1024, gKV, gKVB),
                         (wq_v, g * 1024, gQ, gQB))
                for m, (srcv, col0, gn, gnB) in enumerate(specs):
                    s_ = state["ws"] % 2
                    state["ws"] += 1
                    c0 = col0 + 128 * j
                    P.dma("sp", d_ws[s_], ws[s_][:], srcv[:, :, c0:c0 + 128], w=[wsB[s_]])
                    P.op("pool", lambda e, s_=s_, g=g, m=m, gn=gn: e.tensor_tensor(
                        out=wbf[g * 3 + m][:], in0=ws[s_][:],
                        in1=gn[:, :].unsqueeze(2).to_broadcast([128, KD, 128]), op=ALU.mult),
                        r=[wsB[s_], gnB], w=[wbfB[g * 3 + m]])

        VT = st.enter_context(nc.sbuf_tensor("VTb", [128, S], BF16))
        vtB = Buf()

        def project_pair():
            for g in range(3):
                dil, nblk = _DILS[g], _NBLK[g]
                for m, dst, dB, scale in ((0, KT[:, g * 2048:(g + 1) * 2048], ktB[g], 1.0),
                                          (2, QT[:, g * 2048:(g + 1) * 2048], qtB[g], 0.125),
                                          (1, VT[:, :], vtB, 1.0)):
                    for c in range(4):
                        bank = 2 + state["pr"] % 2
                        state["pr"] += 1

                        def mm(e, g=g, m=m, c=c, bank=bank):
                            ins = None
                            for k in range(KD):
                                ins = e.matmul(C.ps[bank][:, :], wbf[g * 3 + m][:, k, :],
                                               XT[:, k, c * 512:(c + 1) * 512],
                                               start=(k == 0), stop=(k == KD - 1))
                            return ins
                        P.op("pe", mm, r=[wbfB[g * 3 + m], XTB[c]], w=[C.psB[bank]])
                        src = C.ps[bank][:, :]
                        if dil == 1:
                            o = dst[:, c * 512:(c + 1) * 512]
                        elif dil == 4:
                            o = dst.rearrange("p (r n a) -> p r n a", r=4, n=4)[:, :, c, :]
                            src = src.rearrange("p (a r) -> p r a", r=4)
                        else:
                            o = dst.rearrange("p (r a) -> p r a", r=16)[:, :, 32 * c:32 * c + 32]
                            src = src.rearrange("p (a r) -> p r a", r=16)
                        P.op("dve", lambda e, o=o, src=src, scale=scale: e.tensor_scalar(
                            out=o, in0=src, scalar1=scale, scalar2=None, op0=ALU.mult),
                            r=[C.psB[bank]], pw=[dB])
                for q4 in range(4):
                    bank = 2 + state["pr"] % 2
                    state["pr"] += 1
                    psb = C.ps[bank].bitcast(BF16)

                    def vt(e, q4=q4, psb=psb):
                        ins = None
                        for tt in range(4):
                            kt = q4 * 4 + tt
                            ins = e.transpose(psb[:, tt * 128:(tt + 1) * 128], VT[:, kt * 128:(kt + 1) * 128],
                                              C.ident[:])
                        return ins
                    P.op("pe", vt, r=[vtB, C.identB], w=[C.psB[bank]])
                    pv4 = psb[:, 0:512].rearrange("p (t f) -> p t f", t=4)
                    v4 = Vall[:, g, q4 * 4:(q4 + 1) * 4, :]
                    P.op("dve", lambda e, pv4=pv4, v4=v4: e.tensor_copy(out=v4[:, :, 0:64], in_=pv4[:, :, 0:64]),
                         r=[C.psB[bank]], pw=[vB[g]])
                    P.op("dve", lambda e, pv4=pv4, v4=v4: e.tensor_copy(out=v4[:, :, 128:192], in_=pv4[:, :, 64:128]),
                         r=[C.psB[bank]], pw=[vB[g]])

        batches = _att_batches()

        def attend_head(j, hl, deferred=()):
            slot = j % 2
            hp = slice(0, 64) if hl == 0 else slice(64, 128)
            started = set()
            pend = []
            deferred = list(deferred)
            for bi_, (g, half, tiles) in enumerate(batches):
                dil, nblk = _DILS[g], _NBLK[g]
                tidx = _ETAB.index((g, half))
                sb = state["sr"] % 3
                state["sr"] += 1
                nt_ = len(tiles)

                def qk(e, g=g, half=half, tiles=tiles, sb=sb, dil=dil):
                    ins = None
                    for ti, (r_, n_) in enumerate(tiles):
                        nb_ = _NBLK[g]
                        qs = g * 2048 + (r_ * nb_ + n_) * 128
                        ks = qs if half == 0 else qs - 128
                        ins = e.matmul(C.ps[sb][:, ti * 128:(ti + 1) * 128],
                                       KT[hp, ks:ks + 128], QT[hp, qs:qs + 128],
                                       start=True, stop=True, skip_group_check=True)
                    return ins
                P.op("pe", qk, r=[ktB[g], qtB[g]], w=[C.psB[sb]])
                pi = state["pt"] % 3
                state["pt"] += 1
                pt = pts[pi]
                P.op("act", lambda e, pt=pt, sb=sb, nt_=nt_: e.activation(
                    out=pt[:, 0:nt_ * 128], in_=C.ps[sb][:, 0:nt_ * 128], func=AF.Exp),
                    r=[C.psB[sb]], w=[ptB[pi]])
                pv_ = pt[:, 0:nt_ * 128].rearrange("p (t a) -> p t a", t=nt_)
                P.op("dve", lambda e, pv_=pv_, nt_=nt_, tidx=tidx: e.tensor_tensor(
                    out=pv_, in0=pv_,
                    in1=et[slot][:, hl * 5 + tidx, :].unsqueeze(1).to_broadcast([128, nt_, 128]),
                    op=ALU.mult), r=[etB[slot]], w=[ptB[pi]])

                def pv(e, g=g, half=half, tiles=tiles, pt=pt, nblk=nblk):
                    ins = None
                    for ti, (r_, n_) in enumerate(tiles):
                        kt = r_ * nblk + (n_ if half == 0 else n_ - 1)
                        lhsT = Vall[:, g, kt, 0:128] if hl == 0 else Vall[:, g, kt, 64:192]
                        if g == 0:
                            outs = [(4 + n_ // 4, slice((n_ % 4) * 128, (n_ % 4 + 1) * 128),
                                     slice(ti * 128, (ti + 1) * 128))]
                        elif g == 1:
                            outs = [(4 + n_, slice(r_, 512, 4), slice(ti * 128, (ti + 1) * 128))]
                        else:
                            outs = [(4 + b, slice(r_, 512, 16), slice(ti * 128 + 32 * b, ti * 128 + 32 * b + 32))
                                    for b in range(4)]
                        for (bk, osl, rsl) in outs:
                            first = bk not in started
                            started.add(bk)
                            ins = e.matmul(C.ps[bk][:, osl], lhsT, pt[:, rsl], start=first, stop=True,
                                           skip_group_check=True)
                    return ins
                if deferred and bi_ % 4 == 2:
                    deferred.pop(0)()
                pend.append((pv, [ptB[pi], vB[g]]))
                if len(pend) > 2:
                    f_, r_ = pend.pop(0)
                    P.op("pe", f_, r=r_, pw=C.psB[4:8])
            for f_, r_ in pend:
                P.op("pe", f_, r=r_, pw=C.psB[4:8])
            if hl == 0:
                num, den = slice(0, 64), slice(64, 128)
            else:
                num, den = slice(64, 128), slice(0, 64)
            for b in range(4):
                cs = slice(b * 512, (b + 1) * 512)
                P.op("act", lambda e, b=b, cs=cs: e.activation(out=VT[num, cs], in_=C.ps[4 + b][num, :],
                                                               func=AF.Copy, scale=gone[num, 0:1]),
                     r=[C.psB[4 + b], goneB], pw=[vtB])
                P.op("dve", lambda e, b=b, cs=cs: e.tensor_copy(out=rc[num, cs], in_=C.ps[4 + b][den, :]),
                     r=[C.psB[4 + b]], pw=[rcB])
            while deferred:
                deferred.pop(0)()
            mine = []
            for b in range(4):
                def nrm(b=b, num=num):
                    cs = slice(b * 512, (b + 1) * 512)
                    P.op("dve", lambda e: e.reciprocal(out=rc[num, cs], in_=rc[num, cs]),
                         r=[rcB], w=[rcB2[b]])
                    P.op("dve", lambda e: e.tensor_tensor(out=OT[num, cs], in0=VT[num, cs], in1=rc[num, cs],
                                                          op=ALU.mult),
                         r=[vtB, rcB, rcB2[b]], pw=[otB[b]])
                mine.append(nrm)
            return mine

        def out_proj(j, deferred=()):
            slot = j % 2
            deferred = list(deferred)
            for ti in range(NT):
                if ti % 4 == 0 and deferred:
                    deferred.pop(0)()
                for half in range(2):
                    bank = 2 + state["pr"] % 2
                    state["pr"] += 1
                    P.op("pe", lambda e, ti=ti, half=half, bank=bank: e.matmul(
                        C.ps[bank][:, :], OT[:, ti * 128:(ti + 1) * 128],
                        wo[slot][:, half * 512:(half + 1) * 512], start=True, stop=True),
                        r=[otB[ti // 4], woB[slot]], w=[C.psB[bank]])
                    hs = C.h[:, ti, half * 512:(half + 1) * 512]
                    P.op("dve", lambda e, hs=hs, bank=bank: e.tensor_tensor(
                        out=hs, in0=C.ps[bank][:, :], in1=hs, op=ALU.add),
                        r=[C.psB[bank]], w=[C.hB[ti]])

        npairs = getattr(C, "dbg_pairs", 8)
        if getattr(C, "dbg_stop", None) == "xt":
            return
        load_pair_inputs(0)
        load_pair_weights(0)
        if getattr(C, "dbg_stop", None) == "loads":
            return
        for j in range(npairs):
            project_pair()
            if getattr(C, "dbg_stop", None) == "proj":
                return
            if j + 1 < npairs:
                load_pair_inputs(j + 1)
            dA = attend_head(j, 0)
            dB = attend_head(j, 1, dA)
            if j + 1 < npairs:
                load_pair_weights(j + 1)
            out_proj(j, dB)

def phase_c(C, dram, out_ap):
    nc, P = C.nc, C.P
    with contextlib.ExitStack() as st:
        _const_begin(C, "d_constC")
        gF, gFB = _load_gain_cols(C, "gF1", dram["ffn_norm1"])
        fng, fngB = _load_bcast_row(C, st, "fng", dram["final_norm"])
        xh = st.enter_context(nc.sbuf_tensor("xhc", [128, NT, D], BF16))
        xhB = [Buf() for _ in range(NT)]
        XT = st.enter_context(nc.sbuf_tensor("XTc", [128, KD, S], BF16))
        XTB = [Buf() for _ in range(4)]
        wr = st.enter_context(nc.sbuf_tensor("wr", [128, KD, NEXP], BF16))
        wrB = Buf()
        P.dma("pool", C.d_const, wr[:], dram["moe_router"].rearrange("(k p) e -> p k e", p=128),
              w=[wrB])
        C.const_bufs.append(wrB)
        _const_fence(C)
        _rms_stats(C)
        _normalize_tm(C, xh, xhB)
        _transpose_gain(C, xh, xhB, XT, XTB, gF, gFB)
        L = st.enter_context(nc.sbuf_tensor("L", [128, NT, NEXP], F32))
        L2 = st.enter_context(nc.sbuf_tensor("L2", [128, NT, NEXP], F32))
        gate = st.enter_context(nc.sbuf_tensor("gate", [128, NT, NEXP], F32))
        m1 = st.enter_context(nc.sbuf_tensor("m1", [128, NT], F32))
        m2 = st.enter_context(nc.sbuf_tensor("m2", [128, NT], F32))
        LB, L2B, gateB, m1B, m2B = Buf(), Buf(), Buf(), Buf(), Buf()

        def rt(e):
            ins = None
            for i in range(NT):
                for k in range(KD):
                    ins = e.matmul(C.ps[0][:, i * NEXP:(i + 1) * NEXP], XT[:, k, i * 128:(i + 1) * 128],
                                   wr[:, k, :], start=(k == 0), stop=(k == KD - 1),
                                   skip_group_check=True)
            return ins
        P.op("pe", rt, r=XTB + [wrB], w=[C.psB[0]])
        psl = C.ps[0][:, 0:NT * NEXP].rearrange("p (i e) -> p i e", e=NEXP)
        P.op("dve", lambda e: e.tensor_copy(out=L[:], in_=psl), r=[C.psB[0]], w=[LB])
        P.op("dve", lambda e: e.tensor_reduce(out=m1[:], in_=L[:], axis=AX.X, op=ALU.max),
             r=[LB], w=[m1B])
        bc = lambda t: t[:, :].unsqueeze(2).to_broadcast([128, NT, NEXP])
        P.op("dve", lambda e: e.tensor_tensor(out=L2[:], in0=L[:], in1=bc(m1), op=ALU.is_equal),
             r=[LB, m1B], w=[L2B])
        P.op("dve", lambda e: e.scalar_tensor_tensor(out=L2[:], in0=L2[:], scalar=-1e30, in1=L[:],
                                                     op0=ALU.mult, op1=ALU.add),
             r=[LB], w=[L2B])
        P.op("dve", lambda e: e.tensor_reduce(out=m2[:], in_=L2[:], axis=AX.X, op=ALU.max),
             r=[L2B], w=[m2B])
        P.op("dve", lambda e: e.tensor_tensor(out=L2[:], in0=L[:], in1=bc(m2), op=ALU.is_ge),
             r=[LB, m2B], w=[L2B])
        P.op("dve", lambda e: e.tensor_tensor(out=gate[:], in0=L[:], in1=bc(m1), op=ALU.subtract),
             r=[LB, m1B], w=[gateB])
        P.op("act", lambda e: e.activation(out=gate[:], in_=gate[:], func=AF.Exp),
             r=[], w=[gateB])
        P.op("dve", lambda e: e.tensor_tensor(out=gate[:], in0=gate[:], in1=L2[:], op=ALU.mult),
             r=[L2B], w=[gateB])
        P.op("dve", lambda e: e.tensor_reduce(out=m1[:], in_=gate[:], axis=AX.X, op=ALU.add),
             r=[gateB], w=[m1B])
        P.op("dve", lambda e: e.reciprocal(out=m1[:], in_=m1[:]), r=[], w=[m1B])
        P.op("dve", lambda e: e.tensor_tensor(out=gate[:], in0=gate[:], in1=bc(m1), op=ALU.mult),
             r=[m1B], w=[gateB])
        _alloc_ffn(C, st, "C")
        for ex in range(NEXP):
            _ffn(C, st, XT, XTB, dram["moe_w_gate"][ex], dram["moe_w_up"][ex], dram["moe_w_down"][ex],
                 FF_EXP, "e%d" % ex, gate=gate, gateB=gateB, gate_e=ex)
        _rms_stats(C)
        ov = out_ap.rearrange("(n p) d -> p n d", p=128)
        for i in range(NT):
            P.op("dve", lambda e, i=i: e.scalar_tensor_tensor(
                out=C.h[:, i, :], in0=C.h[:, i, :], scalar=C.rstd[:, i:i + 1], in1=fng[:],
                op0=ALU.mult, op1=ALU.mult), r=[C.rstdB, fngB], w=[C.hB[i]])
            if i % 4 == 3:
                tok = P.dma("sp", C.d_io, ov[:, i - 3:i + 1, :], C.h[:, i - 3:i + 1, :],
                            r=C.hB[i - 3:i + 1])
                P.out_toks.append(tok)


def phase_c_sparse(C, dram, out_ap):
    nc, P = C.nc, C.P
    with contextlib.ExitStack() as st:
        _const_begin(C, "d_constC")
        gF, gFB = _load_gain_cols(C, "gF1", dram["ffn_norm1"])
        fng, fngB = _load_bcast_row(C, st, "fng", dram["final_norm"])
        wr = st.enter_context(nc.sbuf_tensor("wr", [128, KD, NEXP], BF16))
        wrB = Buf()
        P.dma("pool", C.d_const, wr[:], dram["moe_router"].rearrange("(k p) e -> p k e", p=128), w=[wrB])
        C.const_bufs.append(wrB)
        iota = st.enter_context(nc.sbuf_tensor("iota", [128, CAP], F32))
        iotaB = Buf()
        P.dma("sp", C.d_const, iota[:], dram["c_iota"], w=[iotaB])
        C.const_bufs.append(iotaB)
        tri = st.enter_context(nc.sbuf_tensor("tri", [128, 128], BF16))
        triB = Buf()
        P.dma("pool", C.d_const, tri[:], dram["c_tri"], w=[triB])
        C.const_bufs.append(triB)
        _const_fence(C)
        ones = st.enter_context(nc.sbuf_tensor("onesb", [128, 128], BF16))
        onesB = Buf()
        P.op("dve", lambda e: e.memset(ones[:], 1.0), w=[onesB])
        onec = st.enter_context(nc.sbuf_tensor("onec", [128, 1], F32))
        onecB = Buf()
        P.op("dve", lambda e: e.memset(onec[:], 1.0), w=[onecB])

        xh = st.enter_context(nc.sbuf_tensor("xhc", [128, NT, D], BF16))
        xhB = [Buf() for _ in range(NT)]
        L = st.enter_context(nc.sbuf_tensor("L", [128, NT, NEXP], F32))
        L2 = st.enter_context(nc.sbuf_tensor("L2", [128, NT, NEXP], F32))
        gate = st.enter_context(nc.sbuf_tensor("gate", [128, NT, NEXP], F32))
        rank = st.enter_context(nc.sbuf_tensor("rank", [128, NT, NEXP], F32))
        tot = st.enter_context(nc.sbuf_tensor("tot", [128, NT, NEXP], F32))
        off = st.enter_context(nc.sbuf_tensor("off", [128, NT, NEXP], F32))
        maskb = st.enter_context(nc.sbuf_tensor("maskb", [128, NT, NEXP], BF16))
        m1 = st.enter_context(nc.sbuf_tensor("m1", [128, NT], F32))
        m2 = st.enter_context(nc.sbuf_tensor("m2", [128, NT], F32))
        LB, L2B, gateB, m1B, m2B, rankB, totB, offB, maskbB = (Buf() for _ in range(9))

        with contextlib.ExitStack() as st2:
            XT = st2.enter_context(nc.sbuf_tensor("XTc", [128, KD, S], BF16))
            XTB = [Buf() for _ in range(4)]
            _rms_stats(C)
            _normalize_tm(C, xh, xhB)
            _transpose_gain(C, xh, xhB, XT, XTB, gF, gFB)

            def rt(e):
                ins = None
                for i in range(NT):
                    for k in range(KD):
                        ins = e.matmul(C.ps[0][:, i * NEXP:(i + 1) * NEXP], XT[:, k, i * 128:(i + 1) * 128],
                                       wr[:, k, :], start=(k == 0), stop=(k == KD - 1),
                                       skip_group_check=True)
                return ins
            P.op("pe", rt, r=XTB + [wrB], w=[C.psB[0]])
            psl = C.ps[0][:, 0:NT * NEXP].rearrange("p (i e) -> p i e", e=NEXP)
            P.op("dve", lambda e: e.tensor_copy(out=L[:], in_=psl), r=[C.psB[0]], w=[LB])
        P.barrier()

        bc = lambda t: t[:, :].unsqueeze(2).to_broadcast([128, NT, NEXP])
        P.op("dve", lambda e: e.tensor_reduce(out=m1[:], in_=L[:], axis=AX.X, op=ALU.max), r=[LB], w=[m1B])
        P.op("dve", lambda e: e.tensor_tensor(out=L2[:], in0=L[:], in1=bc(m1), op=ALU.is_equal),
             r=[LB, m1B], w=[L2B])
        P.op("dve", lambda e: e.scalar_tensor_tensor(out=L2[:], in0=L2[:], scalar=-1e30, in1=L[:],
                                                     op0=ALU.mult, op1=ALU.add), r=[LB], w=[L2B])
        P.op("dve", lambda e: e.tensor_reduce(out=m2[:], in_=L2[:], axis=AX.X, op=ALU.max), r=[L2B], w=[m2B])
        P.op("dve", lambda e: e.tensor_tensor(out=L2[:], in0=L[:], in1=bc(m2), op=ALU.is_ge),
             r=[LB, m2B], w=[L2B])
        P.op("dve", lambda e: e.tensor_tensor(out=gate[:], in0=L[:], in1=bc(m1), op=ALU.subtract),
             r=[LB, m1B], w=[gateB])
        P.op("act", lambda e: e.activation(out=gate[:], in_=gate[:], func=AF.Exp), r=[], w=[gateB])
        P.op("dve", lambda e: e.tensor_tensor(out=gate[:], in0=gate[:], in1=L2[:], op=ALU.mult),
             r=[L2B], w=[gateB])
        P.op("dve", lambda e: e.tensor_reduce(out=m1[:], in_=gate[:], axis=AX.X, op=ALU.add),
             r=[gateB], w=[m1B])
        P.op("dve", lambda e: e.reciprocal(out=m1[:], in_=m1[:]), r=[], w=[m1B])
        P.op("dve", lambda e: e.tensor_tensor(out=gate[:], in0=gate[:], in1=bc(m1), op=ALU.mult),
             r=[m1B], w=[gateB])
        P.op("dve", lambda e: e.tensor_copy(out=maskb[:], in_=L2[:]), r=[L2B], w=[maskbB])
        mflat = maskb[:, :, :].rearrange("p i e -> p (i e)")
        P.op("pe", lambda e: e.matmul(C.ps[0][:, 0:128], tri[:], mflat, start=True, stop=True),
             r=[triB, maskbB], w=[C.psB[0]])
        P.op("pe", lambda e: e.matmul(C.ps[1][:, 0:128], ones[:], mflat, start=True, stop=True),
             r=[onesB, maskbB], w=[C.psB[1]])
        v3 = lambda ps_: ps_[:, 0:128].rearrange("p (i e) -> p i e", e=NEXP)
        P.op("dve", lambda e: e.tensor_copy(out=rank[:], in_=v3(C.ps[0])), r=[C.psB[0]], w=[rankB])
        P.op("dve", lambda e: e.tensor_copy(out=tot[:], in_=v3(C.ps[1])), r=[C.psB[1]], w=[totB])
        P.op("dve", lambda e: e.memset(off[:], 0.0), w=[offB])
        for i in range(1, NT):
            P.op("dve", lambda e, i=i: e.tensor_tensor(out=off[:, i, :], in0=off[:, i - 1, :],
                                                       in1=tot[:, i - 1, :], op=ALU.add),
                 r=[totB], w=[offB])
        P.op("dve", lambda e: e.tensor_tensor(out=rank[:], in0=rank[:], in1=off[:], op=ALU.add),
             r=[offB], w=[rankB])

        SelT = st.enter_context(nc.sbuf_tensor("SelT", [128, NCT, S], BF16))
        selTB = Buf()
        selr = [st.enter_context(nc.sbuf_tensor("selr%d" % i, [128, CAP], BF16)) for i in range(4)]
        selB = [Buf() for _ in range(4)]
        XTe = st.enter_context(nc.sbuf_tensor("XTe", [128, KD, CAP], BF16))
        XTeB = [Buf(), Buf()]
        ye = st.enter_context(nc.sbuf_tensor("ye", [128, NCT, D], F32))
        yeB = [Buf() for _ in range(NCT)]
        yb = st.enter_context(nc.sbuf_tensor("ybf", [128, NCT, D], BF16))
        ybB = [Buf() for _ in range(NCT)]
        _alloc_ffn(C, st, "C", gw=256)
        chunks = [(0, 384), (384, 256)]
        rot = {"s": 0, "t": 0, "y": 0}

        for ex in range(NEXP):
            for kh in range(2):
                for i in range(NT):
                    si = rot["s"] % 4
                    rot["s"] += 1
                    sel = selr[si]
                    P.op("dve", lambda e, i=i, sel=sel, ex=ex: e.tensor_scalar(
                        out=sel[:], in0=iota[:], scalar1=rank[:, i, ex:ex + 1], scalar2=L2[:, i, ex:ex + 1],
                        op0=ALU.is_equal, op1=ALU.mult), r=[iotaB, rankB, L2B], w=[selB[si]])
                    if kh == 0:
                        tb = 5 + rot["t"] % 2
                        rot["t"] += 1
                        psb = C.ps[tb].bitcast(BF16)

                        def trs(e, sel=sel, psb=psb):
                            ins = None
                            for cc in range(NCT):
                                ins = e.transpose(psb[:, cc * 128:(cc + 1) * 128], sel[:, cc * 128:(cc + 1) * 128],
                                                  C.ident[:])
                            return ins
                        P.op("pe", trs, r=[selB[si], C.identB], w=[C.psB[tb]])
                        srcv = psb[:, 0:CAP].rearrange("p (c t) -> p c t", c=NCT)

                        def evs(e, i=i, srcv=srcv):
                            ins = None
                            for cc in range(NCT):
                                ins = e.activation(out=SelT[:, cc, i * 128:(i + 1) * 128], in_=srcv[:, cc, :],
                                                   func=AF.Copy, scale=onec[:, 0:1])
                            return ins
                        P.op("act", evs, r=[C.psB[tb], onecB], pw=[selTB])

                    def ga(e, i=i, kh=kh, sel=sel):
                        ins = None
                        for kk in range(4):
                            k = kh * 4 + kk
                            lt = xh[:, i, k * 128:(k + 1) * 128]
                            e.matmul(C.ps[kk][:, :], lt, sel[:, 0:512], start=(i == 0), stop=(i == NT - 1),
                                     skip_group_check=True)
                            ins = e.matmul(C.ps[4][:, kk * 128:(kk + 1) * 128], lt, sel[:, 512:CAP],
                                           start=(i == 0 and kk == 0), stop=(i == NT - 1), skip_group_check=True)
                        return ins
                    if i == 0:
                        P.op("pe", ga, r=[xhB[i], selB[si]], w=C.psB[0:5])
                    else:
                        P.op("pe", ga, r=[xhB[i], selB[si]], pw=C.psB[0:5])
                for kk in range(4):
                    k = kh * 4 + kk
                    P.op("act", lambda e, k=k, kk=kk: e.activation(
                        out=XTe[:, k, 0:512], in_=C.ps[kk][:, :], func=AF.Copy, scale=gF[:, k:k + 1]),
                        r=[C.psB[kk], gFB], pw=XTeB)
                    P.op("act", lambda e, k=k, kk=kk: e.activation(
                        out=XTe[:, k, 512:CAP], in_=C.ps[4][:, kk * 128:(kk + 1) * 128], func=AF.Copy,
                        scale=gF[:, k:k + 1]), r=[C.psB[4], gFB], pw=XTeB)
            for cc in range(NCT):
                P.op("pool", lambda e, cc=cc: e.memset(ye[:, cc, :], 0.0), w=[yeB[cc]])
            _ffn(C, st, XTe, XTeB, dram["moe_w_gate"][ex], dram["moe_w_up"][ex], dram["moe_w_down"][ex],
                 FF_EXP, "e%d" % ex, chunks=chunks, tgt=ye, tgtB=yeB)
            for cc in range(NCT):
                P.op("act", lambda e, cc=cc: e.activation(out=yb[:, cc, :], in_=ye[:, cc, :], func=AF.Copy,
                                                          scale=onec[:, 0:1]),
                     r=[yeB[cc], onecB], w=[ybB[cc]])
            for i in range(NT):
                for half in range(2):
                    bk = 4 + rot["y"] % 4
                    rot["y"] += 1

                    def sc(e, i=i, half=half, bk=bk):
                        ins = None
                        for cc in range(NCT):
                            ins = e.matmul(C.ps[bk][:, :], SelT[:, cc, i * 128:(i + 1) * 128],
                                           yb[:, cc, half * 512:(half + 1) * 512],
                                           start=(cc == 0), stop=(cc == NCT - 1))
                        return ins
                    P.op("pe", sc, r=[selTB] + ybB, w=[C.psB[bk]])
                    hs = C.h[:, i, half * 512:(half + 1) * 512]
                    gap = gate[:, i, ex:ex + 1]
                    P.op("dve", lambda e, bk=bk, hs=hs, gap=gap: e.scalar_tensor_tensor(
                        out=hs, in0=C.ps[bk][:, :], scalar=gap, in1=hs, op0=ALU.mult, op1=ALU.add),
                        r=[C.psB[bk], gateB], w=[C.hB[i]])

        _rms_stats(C)
        ov = out_ap.rearrange("(n p) d -> p n d", p=128)
        for i in range(NT):
            P.op("dve", lambda e, i=i: e.scalar_tensor_tensor(
                out=C.h[:, i, :], in0=C.h[:, i, :], scalar=C.rstd[:, i:i + 1], in1=fng[:],
                op0=ALU.mult, op1=ALU.mult), r=[C.rstdB, fngB], w=[C.hB[i]])
            if i % 4 == 3:
                tok = P.dma("sp", C.d_io, ov[:, i - 3:i + 1, :], C.h[:, i - 3:i + 1, :],
                            r=C.hB[i - 3:i + 1])
                P.out_toks.append(tok)

_SPECS = {
    "x": [S, D], "a_norm": [D], "a_proj": [4, 256, 256], "a_scale": [D], "kv_norm": [D],
    "w_kv": [D, 6144], "b_norm": [D], "w_q": [D, 3072], "w_o": [D, D], "rel_bias": [32, 48],
    "ffn_norm0": [D], "ffn_norm1": [D], "dense_w_gate": [D, FF_DENSE], "dense_w_up": [D, FF_DENSE],
    "dense_w_down": [FF_DENSE, D], "moe_router": [D, NEXP], "moe_w_gate": [NEXP, D, FF_EXP],
    "moe_w_up": [NEXP, D, FF_EXP], "moe_w_down": [NEXP, FF_EXP, D], "final_norm": [D],
    "c_ident": [128, 128], "c_band": [4, 3, 128, 128], "c_oh": [3, 64, 512], "c_iota": [128, CAP], "c_tri": [128, 128],
}


def build(phases, in_names, hin="x", dbg_stop=None):
    nc = bass.Bass("TRN2", target_bir_lowering=False)
    dram = {}
    for n in in_names:
        dram[n] = nc.dram_tensor(n, _SPECS.get(n, [S, D]), F32, kind="ExternalInput").ap()
    out = nc.dram_tensor("out", [S, D], F32, kind="ExternalOutput").ap()
    with contextlib.ExitStack() as st:
        P = Prog(nc, st)
        C = _alloc_common(nc, st, P)
        C.dbg_stop = dbg_stop
        _const_begin(C, "d_const0")
        _load_ident(C, dram)
        _const_fence(C)
        _load_h(C, dram[hin])
        last = phases[-1]
        for pi_, ph in enumerate(phases):
            if pi_ > 0:
                P.barrier()
            if ph == "a":
                phase_a(C, dram)
            elif ph == "b":
                phase_b(C, dram)
            elif ph == "c":
                phase_c_sparse(C, dram, out)
            elif ph == "cd":
                phase_c(C, dram, out)
        if last != "c":
            _store_h(C, out)
        P.finish()
    return nc


_IN_A = ["x", "a_norm", "a_proj", "a_scale", "ffn_norm0", "dense_w_gate", "dense_w_up",
         "dense_w_down", "c_ident", "c_band"]
_IN_B = ["hin", "kv_norm", "w_kv", "b_norm", "w_q", "w_o", "rel_bias", "c_ident", "c_oh"]
_IN_C = ["hin", "ffn_norm1", "moe_router", "moe_w_gate", "moe_w_up", "moe_w_down", "final_norm",
         "c_ident", "c_iota", "c_tri"]


def _prep(inputs):
    f = lambda a: np.ascontiguousarray(np.asarray(a, dtype=np.float32))
    d = {
        "a_norm": f(inputs["a_norm"][0]), "a_proj": f(inputs["a_proj"][0]),
        "a_scale": f(inputs["a_scale"][0]), "kv_norm": f(inputs["kv_norm"]),
        "w_kv": f(inputs["w_kv"]), "b_norm": f(inputs["b_norm"][0]), "w_q": f(inputs["w_q"][0]),
        "w_o": f(inputs["w_o"][0]), "rel_bias": f(inputs["rel_bias"]),
        "ffn_norm0": f(inputs["ffn_norm"][0]), "ffn_norm1": f(inputs["ffn_norm"][1]),
        "dense_w_gate": f(inputs["dense_w_gate"][0]), "dense_w_up": f(inputs["dense_w_up"][0]),
        "dense_w_down": f(inputs["dense_w_down"][0]), "moe_router": f(inputs["moe_router"][0]),
        "moe_w_gate": f(inputs["moe_w_gate"][0]), "moe_w_up": f(inputs["moe_w_up"][0]),
        "moe_w_down": f(inputs["moe_w_down"][0]), "final_norm": f(inputs["final_norm"]),
    }
    d.update(_host_consts())
    return d


def _run(nc, names, shared, per_core_key, per_core_vals):
    in_maps = []
    for c in range(NCORES):
        m = {n: shared[n] for n in names if n != per_core_key}
        m[per_core_key] = per_core_vals[c]
        in_maps.append(m)
    res = run_bass_kernel_spmd(nc, in_maps, core_ids=list(range(NCORES)))
    return [np.asarray(r["out"]) for r in res.results]


_IN_ALL = ["x", "a_norm", "a_proj", "a_scale", "kv_norm", "w_kv", "b_norm", "w_q", "w_o", "rel_bias",
           "ffn_norm0", "ffn_norm1", "dense_w_gate", "dense_w_up", "dense_w_down", "moe_router",
           "moe_w_gate", "moe_w_up", "moe_w_down", "final_norm", "c_ident", "c_band", "c_oh", "c_iota",
           "c_tri"]


def kernel(**inputs):
    shared = _prep(inputs)
    x = np.ascontiguousarray(np.asarray(inputs["x"], dtype=np.float32))
    xs = [x[b] for b in range(NCORES)]
    nc = build(["a", "b", "c"], _IN_ALL, hin="x")
    h = _run(nc, _IN_ALL, shared, "x", xs)
    return np.stack(h, axis=0).astype(np.float32)
```

```python
import contextlib
import math
import numpy as np
import concourse.bass as bass
import concourse.mybir as mybir
from concourse.bass_utils import run_bass_kernel_spmd

F32 = mybir.dt.float32
BF16 = mybir.dt.bfloat16
AF = mybir.ActivationFunctionType
ALU = mybir.AluOpType
AX = mybir.AxisListType

NCORES = 8
S = 2048
D = 1024
NT = S // 128
KD = D // 128
EPS = 1e-6
FF_DENSE = 2816
FF_EXP = 3584
NEXP = 8
POOL_W = (2, 4, 8, 16)
DIL = ((128, 1), (512, 4), (2048, 16))
NEG = -30000.0
CAP = 640
NCT = CAP // 128


class Buf:
    __slots__ = ("w", "r", "name", "nfull")

    def __init__(self, name=""):
        self.w = []
        self.r = []
        self.nfull = 0
        self.name = name


class Prog:
    def __init__(self, nc, st):
        self.nc = nc
        self.st = st
        self.eng = {"pe": nc.tensor, "act": nc.scalar, "dve": nc.vector,
                    "pool": nc.gpsimd, "sp": nc.sync}
        self.sems = {}
        self.cnt = {}
        for e in ("pe", "act", "dve", "pool"):
            self.sems[e] = st.enter_context(nc.semaphore("s_" + e))
            self.cnt[e] = 0
        self.seen = {e: {} for e in self.eng}
        self.out_toks = []

    def dsem(self, name):
        self.sems[name] = self.st.enter_context(self.nc.semaphore(name))
        self.cnt[name] = 0
        return name

    def _wait(self, e, toks):
        need = {}
        for t in toks:
            key, val, src = t
            if src == "pe" and e == "pe":
                continue
            if need.get(key, 0) < val:
                need[key] = val
        seen = self.seen[e]
        for key, val in need.items():
            if seen.get(key, 0) >= val:
                continue
            seen[key] = val
            self.eng[e].wait_ge(self.sems[key], val)

    def _deps(self, r, w, pw):
        toks = []
        for b in r:
            toks += b.w
        for b in w:
            toks += b.w
            toks += b.r
        for b in pw:
            toks += b.w[:b.nfull]
            toks += b.r
        return toks

    def _upd(self, tok, r, w, pw):
        for b in r:
            b.r.append(tok)
        for b in w:
            b.w = [tok]
            b.nfull = 1
            b.r = []
        for b in pw:
            b.w.append(tok)

    def op(self, e, fn, r=(), w=(), pw=()):
        self._wait(e, self._deps(r, w, pw))
        ins = fn(self.eng[e])
        self.cnt[e] += 1
        ins.then_inc(self.sems[e], 1)
        tok = (e, self.cnt[e], e)
        self._upd(tok, r, w, pw)
        return tok

    def dma(self, q, dkey, out, in_, r=(), w=(), pw=(), **kw):
        self._wait(q, self._deps(r, w, pw))
        ins = self.eng[q].dma_start(out=out, in_=in_, **kw)
        self.cnt[dkey] += 16
        ins.then_inc(self.sems[dkey], 16)
        tok = (dkey, self.cnt[dkey], "dma")
        self._upd(tok, r, w, pw)
        return tok

    def barrier(self):
        toks = [(k, v, "bar") for k, v in self.cnt.items() if v > 0]
        for e in self.eng:
            self._wait(e, toks)

    def finish(self):
        self._wait("sp", self.out_toks)


def _t5_bucket(n):
    n = np.asarray(n, np.int64)
    nf = np.maximum(n, 1).astype(np.float32)
    large = 16 + (np.log(nf / np.float32(16)) / np.float32(math.log(2048 / 16))
                  * np.float32(16)).astype(np.int32)
    large = np.minimum(large, 31)
    return np.where(n < 16, n, large)


def _host_consts():
    ident = np.eye(128, dtype=np.float32)
    band = np.zeros((4, 3, 128, 128), np.float32)
    tp = np.arange(128)[:, None]
    t = np.arange(128)[None, :]
    for g, w in enumerate(POOL_W):
        inwin = (tp <= t) & (tp > t - w)
        cnt0 = np.minimum(t + 1, w).astype(np.float32)
        band[g, 0] = inwin / cnt0 - (tp == t)
        band[g, 1] = inwin / np.float32(w) - (tp == t)
        band[g, 2] = ((tp + 0 > t + 128 - w)) / np.float32(w)
    oh = np.zeros((3, 64, 512), np.float32)
    for g, (_, dil) in enumerate(DIL):
        for idx in range(255):
            m = idx - 127
            if m >= 0:
                oh[g, int(_t5_bucket(m * dil)), idx] = 1.0
            else:
                oh[g, 32, idx] = NEG
            m2 = idx + 1
            if m2 <= 128:
                oh[g, int(_t5_bucket(m2 * dil)), 256 + idx] = 1.0
            else:
                oh[g, 32, 256 + idx] = NEG
        oh[g, 32, 255] = NEG
        oh[g, 32, 511] = NEG
    iota = np.tile(np.arange(CAP, dtype=np.float32)[None], (128, 1))
    tri = (np.arange(128)[:, None] < np.arange(128)[None, :]).astype(np.float32)
    return {"c_ident": ident, "c_band": band, "c_oh": oh, "c_iota": iota, "c_tri": tri}


class Ctx:
    pass


def _alloc_common(nc, st, P):
    C = Ctx()
    C.nc, C.st, C.P = nc, st, P
    C.h = st.enter_context(nc.sbuf_tensor("h", [128, NT, D], F32))
    C.hB = [Buf("h%d" % i) for i in range(NT)]
    C.ps = [st.enter_context(nc.psum_tensor("ps%d" % i, [128, 512], F32)) for i in range(8)]
    C.psB = [Buf("ps%d" % i) for i in range(8)]
    C.ident = st.enter_context(nc.sbuf_tensor("ident", [128, 128], BF16))
    C.identB = Buf("ident")
    C.ss = st.enter_context(nc.sbuf_tensor("ss", [128, NT], F32))
    C.rstd = st.enter_context(nc.sbuf_tensor("rstd", [128, NT], F32))
    C.ssB = Buf("ss")
    C.rstdB = Buf("rstd")
    C.junk = st.enter_context(nc.sbuf_tensor("junk", [128, D], BF16))
    C.junkB = Buf("junk")
    C.d_const = None
    C.const_bufs = []
    C.d_io = P.dsem("d_io")
    return C


def _const_begin(C, name):
    C.d_const = C.P.dsem(name)
    C.const_bufs = []


def _const_fence(C):
    best = None
    for b in C.const_bufs:
        for t in b.w:
            if best is None or t[1] > best[1]:
                best = t
    for b in C.const_bufs:
        b.w = [best]
        b.nfull = 1
    C.const_bufs = []


def _load_ident(C, dram):
    C.P.dma("pool", C.d_const, C.ident[:], dram["c_ident"], w=[C.identB])
    C.const_bufs.append(C.identB)


def _load_gain_cols(C, name, src_ap):
    t = C.st.enter_context(C.nc.sbuf_tensor(name, [128, KD], F32))
    b = Buf(name)
    C.P.dma("sp", C.d_const, t[:], src_ap.rearrange("(k p) -> p k", p=128), w=[b],
            allow_slow_non_contiguous=True)
    C.const_bufs.append(b)
    return t, b


def _load_bcast_row(C, st, name, src_ap):
    t = st.enter_context(C.nc.sbuf_tensor(name, [128, D], F32))
    b = Buf(name)
    C.P.dma("sp", C.d_const, t[:], src_ap.unsqueeze(0).to_broadcast([128, D]), w=[b])
    C.const_bufs.append(b)
    return t, b


def _load_h(C, src):
    v = src.rearrange("(n p) d -> p n d", p=128)
    for i in range(0, NT, 4):
        C.P.dma("sp", C.d_io, C.h[:, i:i + 4, :], v[:, i:i + 4, :], w=C.hB[i:i + 4])
    last = C.hB[NT - 1].w
    for b in C.hB:
        b.w = list(last)
        b.nfull = len(b.w)


def _store_h(C, dst):
    v = dst.rearrange("(n p) d -> p n d", p=128)
    for i in range(0, NT, 4):
        tok = C.P.dma("sp", C.d_io, v[:, i:i + 4, :], C.h[:, i:i + 4, :], r=C.hB[i:i + 4])
        C.P.out_toks.append(tok)


def _rms_stats(C):
    P = C.P
    P.op("dve", lambda e: e.memset(C.ss[:], 0.0), w=[C.ssB])
    for i in range(NT):
        P.op("act", lambda e, i=i: e.activation(out=C.junk[:], in_=C.h[:, i, :], func=AF.Square,
                                                accum_out=C.ss[:, i:i + 1]),
             r=[C.hB[i]], w=[C.junkB], pw=[C.ssB])
    P.op("act", lambda e: e.activation(out=C.rstd[:], in_=C.ss[:], func=AF.Sqrt,
                                       scale=1.0 / D, bias=EPS),
         r=[C.ssB], w=[C.rstdB])
    P.op("dve", lambda e: e.reciprocal(out=C.rstd[:], in_=C.rstd[:]), r=[], w=[C.rstdB])


def _normalize_tm(C, xh, xhB):
    P = C.P
    for i in range(NT):
        if i % 2 == 0:
            P.op("dve", lambda e, i=i: e.tensor_scalar(out=xh[:, i, :], in0=C.h[:, i, :],
                                                       scalar1=C.rstd[:, i:i + 1], scalar2=None,
                                                       op0=ALU.mult),
                 r=[C.hB[i], C.rstdB], w=[xhB[i]])
        else:
            P.op("act", lambda e, i=i: e.activation(out=xh[:, i, :], in_=C.h[:, i, :], func=AF.Copy,
                                                    scale=C.rstd[:, i:i + 1]),
                 r=[C.hB[i], C.rstdB], w=[xhB[i]])


def _transpose_gain(C, xh, xhB, XT, XTB, gcol, gB, col0=0):
    P = C.P
    for i in range(NT):
        bank = i % 2
        psb = C.ps[bank].bitcast(BF16)

        def tr(e, i=i, psb=psb):
            ins = None
            for k in range(KD):
                ins = e.transpose(psb[:, k * 128:(k + 1) * 128], xh[:, i, k * 128:(k + 1) * 128],
                                  C.ident[:])
            return ins
        P.op("pe", tr, r=[xhB[i], C.identB], w=[C.psB[bank]])
        eng = "dve" if i % 2 == 0 else "act"
        src = psb[:, :].rearrange("p (k t) -> p k t", k=KD)
        dst = XT[:, :, col0 + i * 128: col0 + (i + 1) * 128]
        if eng == "dve":
            P.op("dve", lambda e, src=src, dst=dst: e.tensor_tensor(
                out=dst, in0=src, in1=gcol[:, :].unsqueeze(2).to_broadcast([128, KD, 128]),
                op=ALU.mult), r=[C.psB[bank], gB], pw=[XTB[i // 4]])
        else:
            def ev(e, src=src, dst=dst):
                ins = None
                for k in range(KD):
                    ins = e.activation(out=dst[:, k, :], in_=src[:, k, :], func=AF.Copy,
                                       scale=gcol[:, k:k + 1])
                return ins
            P.op("act", ev, r=[C.psB[bank], gB], pw=[XTB[i // 4]])


def _ffn(C, st, XT, XTB, wg, wu, wd, F, tag, gate=None, gateB=None, gate_e=0,
         chunks=None, tgt=None, tgtB=None):
    P, nc = C.P, C.nc
    W = C.ffn_w
    if chunks is None:
        chunks = [(c * 512, 512) for c in range(4)]
    if tgt is None:
        tgt, tgtB = C.h, C.hB
    groups = []
    f0 = 0
    while f0 < F:
        fw = min(W.gw, F - f0)
        groups.append((f0, fw))
        f0 += fw
    wgv = wg.rearrange("(k p) f -> p k f", p=128)
    wuv = wu.rearrange("(k p) f -> p k f", p=128)
    for (f0, fw) in groups:
        nfc = fw // 128
        s = W.slot % 2
        W.slot += 1
        gsb, usb, dsb = W.g[s], W.u[s], W.d[s]
        bG, bU, bD = W.gB[s], W.uB[s], W.dB[s]
        P.dma("pool", W.sem[s], gsb[:, :, 0:fw], wgv[:, :, f0:f0 + fw], w=[bG])
        P.dma("pool", W.sem[s], usb[:, :, 0:fw], wuv[:, :, f0:f0 + fw], w=[bU])
        P.dma("pool", W.sem[s], dsb[:, 0:nfc, :],
              wd[f0:f0 + fw, :].rearrange("(c p) d -> p c d", p=128), w=[bD])
        allw = bG.w + bU.w + bD.w
        for b_ in (bG, bU, bD):
            b_.w = list(allw)
            b_.nfull = len(allw)
        for c, (t0_, tw_) in enumerate(chunks):
            tsl = slice(t0_, t0_ + tw_)
            for fc in range(nfc):
                pg = W.rot % 2
                W.rot += 1
                gi, ui = 2 * pg, 2 * pg + 1

                def mm(e, src, bank, fc=fc, tsl=tsl, tw_=tw_):
                    ins = None
                    for k in range(KD):
                        ins = e.matmul(C.ps[bank][:, 0:tw_], src[:, k, fc * 128:(fc + 1) * 128],
                                       XT[:, k, tsl], start=(k == 0), stop=(k == KD - 1))
                    return ins
                P.op("pe", lambda e, gi=gi, mm=mm: mm(e, gsb, gi), r=[bG, XTB[c]], w=[C.psB[gi]])
                P.op("pe", lambda e, ui=ui, mm=mm: mm(e, usb, ui), r=[bU, XTB[c]], w=[C.psB[ui]])
                sgt = W.sg[pg]
                P.op("act", lambda e, gi=gi, sgt=sgt, tw_=tw_: e.activation(out=sgt[:, 0:tw_], in_=C.ps[gi][:, 0:tw_],
                                                                            func=AF.Silu),
                     r=[C.psB[gi]], w=[W.sgB[pg]])
                P.op("dve", lambda e, ui=ui, sgt=sgt, fc=fc, tw_=tw_: e.tensor_tensor(
                    out=W.ht[:, fc, 0:tw_], in0=sgt[:, 0:tw_], in1=C.ps[ui][:, 0:tw_], op=ALU.mult),
                    r=[W.sgB[pg], C.psB[ui]], w=[W.htB[fc]])
            for half in range(2):
                for tt in range(tw_ // 128):
                    ti = t0_ // 128 + tt
                    yb = 4 + (W.yrot % 4)
                    W.yrot += 1

                    def dn(e, yb=yb, tt=tt, half=half):
                        ins = None
                        for fc in range(nfc):
                            ins = e.matmul(C.ps[yb][:, :], W.ht[:, fc, tt * 128:(tt + 1) * 128],
                                           dsb[:, fc, half * 512:(half + 1) * 512],
                                           start=(fc == 0), stop=(fc == nfc - 1))
                        return ins
                    P.op("pe", dn, r=[bD] + W.htB[0:nfc], w=[C.psB[yb]])
                    hs = tgt[:, ti, half * 512:(half + 1) * 512]
                    if gate is None:
                        P.op("dve", lambda e, yb=yb, hs=hs: e.tensor_tensor(
                            out=hs, in0=C.ps[yb][:, :], in1=hs, op=ALU.add),
                            r=[C.psB[yb]], w=[tgtB[ti]])
                    else:
                        gap = gate[:, ti, gate_e:gate_e + 1]
                        P.op("dve", lambda e, yb=yb, hs=hs, gap=gap: e.scalar_tensor_tensor(
                            out=hs, in0=C.ps[yb][:, :], scalar=gap, in1=hs,
                            op0=ALU.mult, op1=ALU.add),
                            r=[C.psB[yb], gateB], w=[tgtB[ti]])


def _alloc_ffn(C, st, tag="", gw=512):
    nc, P = C.nc, C.P
    W = Ctx()
    W.slot = 0
    W.gw = gw
    W.rot = 0
    W.yrot = 0
    W.g = [st.enter_context(nc.sbuf_tensor(tag + "wg%d" % s, [128, KD, gw], BF16)) for s in range(2)]
    W.u = [st.enter_context(nc.sbuf_tensor(tag + "wu%d" % s, [128, KD, gw], BF16)) for s in range(2)]
    W.d = [st.enter_context(nc.sbuf_tensor(tag + "wd%d" % s, [128, gw // 128, D], BF16)) for s in range(2)]
    W.gB = [Buf() for _ in range(2)]
    W.uB = [Buf() for _ in range(2)]
    W.dB = [Buf() for _ in range(2)]
    W.sem = [P.dsem("d_ffw%s%d" % (tag, s)) for s in range(2)]
    W.sg = [st.enter_context(nc.sbuf_tensor(tag + "sg%d" % s, [128, 512], BF16)) for s in range(2)]
    W.sgB = [Buf() for _ in range(2)]
    W.ht = st.enter_context(nc.sbuf_tensor(tag + "ht", [128, gw // 128, 512], BF16))
    W.htB = [Buf() for _ in range(4)]
    C.ffn_w = W


def phase_a(C, dram):
    nc, P = C.nc, C.P
    with contextlib.ExitStack() as st:
        _const_begin(C, "d_constA")
        gA, gAB = _load_gain_cols(C, "gA", dram["a_norm"])
        gF, gFB = _load_gain_cols(C, "gF0", dram["ffn_norm0"])
        asc, ascB = _load_bcast_row(C, st, "asc", dram["a_scale"])
        xh = st.enter_context(nc.sbuf_tensor("xh", [128, NT, D], BF16))
        xhB = [Buf() for _ in range(NT)]
        XT = st.enter_context(nc.sbuf_tensor("XT", [128, KD, S], BF16))
        XTB = [Buf() for _ in range(4)]
        band = st.enter_context(nc.sbuf_tensor("band", [128, 12, 128], BF16))
        bandB = Buf()
        P.dma("pool", C.d_const, band[:], dram["c_band"].rearrange("g j a b -> a (g j) b"), w=[bandB])
        C.const_bufs.append(bandB)
        pstage = st.enter_context(nc.sbuf_tensor("pstage", [128, 8, 256], F32))
        pw = st.enter_context(nc.sbuf_tensor("pw", [128, 8, 256], BF16))
        pstB, pwB = Buf(), Buf()
        P.dma("sp", C.d_const, pstage[:], dram["a_proj"].rearrange("g (j p) f -> p (g j) f", p=128),
              w=[pstB])
        C.const_bufs.append(pstB)
        _const_fence(C)
        for g in range(4):
            P.op("pool", lambda e, g=g: e.tensor_tensor(
                out=pw[:, 2 * g:2 * g + 2, :], in0=pstage[:, 2 * g:2 * g + 2, :],
                in1=asc[:, g * 256:(g + 1) * 256].unsqueeze(1).to_broadcast([128, 2, 256]),
                op=ALU.mult), r=[pstB, ascB], pw=[pwB])

        _rms_stats(C)
        _normalize_tm(C, xh, xhB)
        for i in range(NT):
            for kk in range(2):
                bank = (2 * i + kk) % 4

                def pm(e, i=i, kk=kk, bank=bank):
                    ins = None
                    for j in range(4):
                        k = kk * 4 + j
                        g = k // 2
                        o = C.ps[bank][:, j * 128:(j + 1) * 128]
                        if i == 0:
                            ins = e.matmul(o, xh[:, 0, k * 128:(k + 1) * 128], band[:, 3 * g + 0, :],
                                           start=True, stop=True, skip_group_check=True)
                        else:
                            e.matmul(o, xh[:, i - 1, k * 128:(k + 1) * 128], band[:, 3 * g + 2, :],
                                     start=True, stop=False, skip_group_check=True)
                            ins = e.matmul(o, xh[:, i, k * 128:(k + 1) * 128], band[:, 3 * g + 1, :],
                                           start=False, stop=True, skip_group_check=True)
                    return ins
                rd = [xhB[i], bandB] + ([xhB[i - 1]] if i > 0 else [])
                P.op("pe", pm, r=rd, w=[C.psB[bank]])
                src = C.ps[bank][:, :].rearrange("p (k t) -> p k t", k=4)
                dst = XT[:, kk * 4:(kk + 1) * 4, i * 128:(i + 1) * 128]
                P.op("dve", lambda e, src=src, dst=dst, kk=kk: e.tensor_tensor(
                    out=dst, in0=src,
                    in1=gA[:, kk * 4:(kk + 1) * 4].unsqueeze(2).to_broadcast([128, 4, 128]),
                    op=ALU.mult), r=[C.psB[bank], gAB], pw=[XTB[i // 4]])
        for i in range(NT):
            for half in range(2):
                bank = 4 + (2 * i + half) % 4

                def pj(e, i=i, half=half, bank=bank):
                    ins = None
                    for gg in range(2):
                        g = half * 2 + gg
                        for j in range(2):
                            ins = e.matmul(C.ps[bank][:, gg * 256:(gg + 1) * 256],
                                           XT[:, 2 * g + j, i * 128:(i + 1) * 128],
                                           pw[:, 2 * g + j, :], start=(j == 0), stop=(j == 1),
                                           skip_group_check=True)
                    return ins
                P.op("pe", pj, r=[XTB[i // 4], pwB], w=[C.psB[bank]])
                hs = C.h[:, i, half * 512:(half + 1) * 512]
                P.op("dve", lambda e, bank=bank, hs=hs: e.tensor_tensor(
                    out=hs, in0=C.ps[bank][:, :], in1=hs, op=ALU.add),
                    r=[C.psB[bank]], w=[C.hB[i]])

        if getattr(C, "dbg_stop", None) == "mixer":
            return
        _rms_stats(C)
        _normalize_tm(C, xh, xhB)
        _transpose_gain(C, xh, xhB, XT, XTB, gF, gFB)
        _alloc_ffn(C, st, "A")
        _ffn(C, st, XT, XTB, dram["dense_w_gate"], dram["dense_w_up"], dram["dense_w_down"],
             FF_DENSE, "dense")


_NBLK = (16, 4, 1)
_DILS = (1, 4, 16)
_ETAB = ((0, 0), (0, 1), (1, 0), (1, 1), (2, 0))


def _att_batches():
    out = []
    out += [(0, 0, [(0, n) for n in range(b, b + 4)]) for b in range(0, 16, 4)]
    prev = [(0, n) for n in range(1, 16)]
    out += [(0, 1, prev[b:b + 4]) for b in range(0, 15, 4)]
    for r in range(4):
        out.append((1, 0, [(r, n) for n in range(4)]))
        out.append((1, 1, [(r, n) for n in range(1, 4)]))
    out += [(2, 0, [(r, 0) for r in range(b, b + 4)]) for b in range(0, 16, 4)]
    return out


def phase_b(C, dram):
    nc, P = C.nc, C.P
    with contextlib.ExitStack() as st:
        _const_begin(C, "d_constB")
        gKV, gKVB = _load_gain_cols(C, "gKV", dram["kv_norm"])
        gQ, gQB = _load_gain_cols(C, "gQ", dram["b_norm"])
        relb = st.enter_context(nc.sbuf_tensor("relb", [64, 48], F32))
        relbB = Buf()
        P.dma("sp", C.d_const, relb[0:32, :], dram["rel_bias"], w=[relbB])
        C.const_bufs.append(relbB)
        oh = st.enter_context(nc.sbuf_tensor("oh", [64, 3, 512], BF16))
        ohB = Buf()
        P.dma("pool", C.d_const, oh[:], dram["c_oh"].rearrange("g b i -> b g i"), w=[ohB])
        C.const_bufs.append(ohB)
        _const_fence(C)
        P.op("dve", lambda e: e.memset(relb[32:64, :], 1.0), pw=[relbB])
        gone = st.enter_context(nc.sbuf_tensor("gone", [128, KD], F32))
        goneB = Buf()
        P.op("dve", lambda e: e.memset(gone[:], 1.0), w=[goneB])
        qsc = st.enter_context(nc.sbuf_tensor("qsc", [128, 1], F32))
        qscB = Buf()
        P.op("dve", lambda e: e.memset(qsc[:], 0.125), w=[qscB])

        XT = st.enter_context(nc.sbuf_tensor("XTb", [128, KD, S], BF16))
        XTB = [Buf() for _ in range(4)]
        big = st.enter_context(nc.sbuf_tensor("bigb", [128, 21504], BF16))
        big_toks = []
        et = [st.enter_context(nc.sbuf_tensor("et%d" % s_, [128, 10, 128], BF16)) for s_ in range(2)]
        etB = [Buf() for _ in range(2)]
        d_et = [P.dsem("d_et%d" % s_) for s_ in range(2)]
        ws = [st.enter_context(nc.sbuf_tensor("wsb%d" % s_, [128, KD, 128], F32)) for s_ in range(2)]
        wsB = [Buf() for _ in range(2)]
        d_ws = [P.dsem("d_ws%d" % s_) for s_ in range(2)]
        wbf = [st.enter_context(nc.sbuf_tensor("wbf%d" % m, [128, KD, 128], BF16)) for m in range(9)]
        wbfB = [Buf() for _ in range(9)]
        pts = [st.enter_context(nc.sbuf_tensor("pt%d" % s_, [128, 512], BF16)) for s_ in range(3)]
        ptB = [Buf() for _ in range(3)]
        rc = st.enter_context(nc.sbuf_tensor("rc", [128, S], F32))
        rcB = Buf()
        rcB2 = [Buf() for _ in range(4)]
        OT = st.enter_context(nc.sbuf_tensor("OT", [128, S], BF16))
        otB = [Buf() for _ in range(4)]
        wo = [st.enter_context(nc.sbuf_tensor("wo%d" % s_, [128, D], BF16)) for s_ in range(2)]
        woB = [Buf() for _ in range(2)]
        d_wo = [P.dsem("d_wo%d" % s_) for s_ in range(2)]
        fv = [st.enter_context(nc.sbuf_tensor("fv%d" % s_, [128, 512], BF16)) for s_ in range(2)]
        fvB = [Buf() for _ in range(2)]
        d_fs = P.dsem("d_fs")
        fscr = nc.dram_tensor("fscr", [48, 128, 512], BF16)
        fscr_ap = fscr.ap()
        fscrB = Buf()

        rhi = st.enter_context(nc.sbuf_tensor("rhi", [64, 48], BF16))
        rlo = st.enter_context(nc.sbuf_tensor("rlo", [64, 48], BF16))
        rhiB, rloB = Buf(), Buf()
        P.op("dve", lambda e: e.tensor_copy(out=rhi[:], in_=relb[:]), r=[relbB], w=[rhiB])
        P.op("dve", lambda e: e.tensor_tensor(out=rlo[:], in0=relb[:], in1=rhi[:], op=ALU.subtract),
             r=[relbB, rhiB], w=[rloB])
        rbc_hi = big[0:64, 0:2048].rearrange("p (h c) -> p h c", c=128)
        rbc_lo = big[0:64, 2048:4096].rearrange("p (h c) -> p h c", c=128)
        relbcB = Buf()
        for g in range(3):
            P.op("dve", lambda e, g=g: e.tensor_copy(
                out=rbc_hi, in_=rhi[:, g * 16:(g + 1) * 16].unsqueeze(2).to_broadcast([64, 16, 128])),
                r=[rhiB], w=[relbcB])
            P.op("dve", lambda e, g=g: e.tensor_copy(
                out=rbc_lo, in_=rlo[:, g * 16:(g + 1) * 16].unsqueeze(2).to_broadcast([64, 16, 128])),
                r=[rloB], pw=[relbcB])
            for hh in range(16):
                gh = g * 16 + hh
                bank = 2 + gh % 2

                def eg(e, g=g, hh=hh, bank=bank):
                    e.matmul(C.ps[bank][:, :], rbc_hi[:, hh, :], oh[:, g, :], start=True, stop=False)
                    return e.matmul(C.ps[bank][:, :], rbc_lo[:, hh, :], oh[:, g, :], start=False, stop=True)
                P.op("pe", eg, r=[relbcB, ohB], w=[C.psB[bank]])
                P.op("act", lambda e, gh=gh, bank=bank: e.activation(
                    out=fv[gh % 2][:], in_=C.ps[bank][:, :], func=AF.Exp),
                    r=[C.psB[bank]], w=[fvB[gh % 2]])
                P.dma("sp", d_fs, fscr_ap[gh], fv[gh % 2][:], r=[fvB[gh % 2]], pw=[fscrB])
        big_toks += relbcB.w + relbcB.r

        xh = big[:, 0:NT * D].rearrange("p (i d) -> p i d", i=NT)
        xhB = [Buf() for _ in range(NT)]
        for b in xhB:
            b.r = list(big_toks)
        _rms_stats(C)
        _normalize_tm(C, xh, xhB)
        _transpose_gain(C, xh, xhB, XT, XTB, gone, goneB)
        for b in xhB:
            big_toks += b.w + b.r

        KT = big[:, 0:6144]
        QT = big[:, 6144:12288]
        Vall = big[:, 12288:21504].rearrange("p (g t c) -> p g t c", g=3, t=16)
        ktB = [Buf() for _ in range(3)]
        qtB = [Buf() for _ in range(3)]
        vB = [Buf() for _ in range(3)]
        for b in ktB + qtB + vB:
            b.r = list(big_toks)
        for g in range(3):
            P.op("dve", lambda e, g=g: e.memset(Vall[:, g, :, 64:128], 1.0), w=[vB[g]])

        wkv_v = dram["w_kv"].rearrange("(k p) f -> p k f", p=128)
        wq_v = dram["w_q"].rearrange("(k p) f -> p k f", p=128)
        state = {"ws": 0, "pr": 0, "sr": 0, "pt": 0}

        def load_pair_inputs(j):
            slot = j % 2
            for hl in range(2):
                hh = 2 * j + hl
                for ti, (g, half) in enumerate(_ETAB):
                    gh = g * 16 + hh
                    src = bass.AP(fscr, gh * 128 * 512 + half * 256 + 127, [[511, 128], [1, 128]])
                    P.dma("sp", d_et[slot], et[slot][:, hl * 5 + ti, :], src, r=[fscrB], pw=[etB[slot]])
            P.dma("pool", d_wo[slot], wo[slot][:], dram["w_o"][128 * j:128 * (j + 1), :], w=[woB[slot]])

        def load_pair_weights(j):
            for g in range(3):
                specs = ((wkv_v, g * 1024, gKV, gKVB), (wkv_v, 3072 + g * 1024, gKV, gKVB),
                         (wq_v, g * 1024, gQ, gQB))
                for m, (srcv, col0, gn, gnB) in enumerate(specs):
                    s_ = state["ws"] % 2
                    state["ws"] += 1
                    c0 = col0 + 128 * j
                    P.dma("sp", d_ws[s_], ws[s_][:], srcv[:, :, c0:c0 + 128], w=[wsB[s_]])
                    P.op("pool", lambda e, s_=s_, g=g, m=m, gn=gn: e.tensor_tensor(
                        out=wbf[g * 3 + m][:], in0=ws[s_][:],
                        in1=gn[:, :].unsqueeze(2).to_broadcast([128, KD, 128]), op=ALU.mult),
                        r=[wsB[s_], gnB], w=[wbfB[g * 3 + m]])

        VT = st.enter_context(nc.sbuf_tensor("VTb", [128, S], BF16))
        vtB = Buf()

        def project_pair():
            for g in range(3):
                dil, nblk = _DILS[g], _NBLK[g]
                for m, dst, dB, scale in ((0, KT[:, g * 2048:(g + 1) * 2048], ktB[g], 1.0),
                                          (2, QT[:, g * 2048:(g + 1) * 2048], qtB[g], 0.125),
                                          (1, VT[:, :], vtB, 1.0)):
                    for c in range(4):
                        bank = 2 + state["pr"] % 2
                        state["pr"] += 1

                        def mm(e, g=g, m=m, c=c, bank=bank):
                            ins = None
                            for k in range(KD):
                                ins = e.matmul(C.ps[bank][:, :], wbf[g * 3 + m][:, k, :],
                                               XT[:, k, c * 512:(c + 1) * 512],
                                               start=(k == 0), stop=(k == KD - 1))
                            return ins
                        P.op("pe", mm, r=[wbfB[g * 3 + m], XTB[c]], w=[C.psB[bank]])
                        src = C.ps[bank][:, :]
                        if dil == 1:
                            o = dst[:, c * 512:(c + 1) * 512]
                        elif dil == 4:
                            o = dst.rearrange("p (r n a) -> p r n a", r=4, n=4)[:, :, c, :]
                            src = src.rearrange("p (a r) -> p r a", r=4)
                        else:
                            o = dst.rearrange("p (r a) -> p r a", r=16)[:, :, 32 * c:32 * c + 32]
                            src = src.rearrange("p (a r) -> p r a", r=16)
                        if (state["pr"] % 2 == 0) and dil == 1:
                            sap = qsc[:, 0:1] if scale != 1.0 else gone[:, 0:1]
                            P.op("act", lambda e, o=o, src=src, sap=sap: e.activation(
                                out=o, in_=src, func=AF.Copy, scale=sap),
                                r=[C.psB[bank], goneB, qscB], pw=[dB])
                        else:
                            P.op("dve", lambda e, o=o, src=src, scale=scale: e.tensor_scalar(
                                out=o, in0=src, scalar1=scale, scalar2=None, op0=ALU.mult),
                                r=[C.psB[bank]], pw=[dB])
                for q4 in range(4):
                    bank = 2 + state["pr"] % 2
                    state["pr"] += 1
                    psb = C.ps[bank].bitcast(BF16)

                    def vt(e, q4=q4, psb=psb):
                        ins = None
                        for tt in range(4):
                            kt = q4 * 4 + tt
                            ins = e.transpose(psb[:, tt * 128:(tt + 1) * 128], VT[:, kt * 128:(kt + 1) * 128],
                                              C.ident[:])
                        return ins
                    P.op("pe", vt, r=[vtB, C.identB], w=[C.psB[bank]])
                    pv4 = psb[:, 0:512].rearrange("p (t f) -> p t f", t=4)
                    v4 = Vall[:, g, q4 * 4:(q4 + 1) * 4, :]
                    P.op("dve", lambda e, pv4=pv4, v4=v4: e.tensor_copy(out=v4[:, :, 0:64], in_=pv4[:, :, 0:64]),
                         r=[C.psB[bank]], pw=[vB[g]])
                    P.op("dve", lambda e, pv4=pv4, v4=v4: e.tensor_copy(out=v4[:, :, 128:192], in_=pv4[:, :, 64:128]),
                         r=[C.psB[bank]], pw=[vB[g]])

        batches = _att_batches()

        def attend_head(j, hl, deferred=()):
            slot = j % 2
            hp = slice(0, 64) if hl == 0 else slice(64, 128)
            started = set()
            pend = []
            deferred = list(deferred)
            for bi_, (g, half, tiles) in enumerate(batches):
                dil, nblk = _DILS[g], _NBLK[g]
                tidx = _ETAB.index((g, half))
                sb = state["sr"] % 3
                state["sr"] += 1
                nt_ = len(tiles)

                def qk(e, g=g, half=half, tiles=tiles, sb=sb, dil=dil):
                    ins = None
                    for ti, (r_, n_) in enumerate(tiles):
                        nb_ = _NBLK[g]
                        qs = g * 2048 + (r_ * nb_ + n_) * 128
                        ks = qs if half == 0 else qs - 128
                        ins = e.matmul(C.ps[sb][:, ti * 128:(ti + 1) * 128],
                                       KT[hp, ks:ks + 128], QT[hp, qs:qs + 128],
                                       start=True, stop=True, skip_group_check=True)
                    return ins
                P.op("pe", qk, r=[ktB[g], qtB[g]], w=[C.psB[sb]])
                pi = state["pt"] % 3
                state["pt"] += 1
                pt = pts[pi]
                P.op("act", lambda e, pt=pt, sb=sb, nt_=nt_: e.activation(
                    out=pt[:, 0:nt_ * 128], in_=C.ps[sb][:, 0:nt_ * 128], func=AF.Exp),
                    r=[C.psB[sb]], w=[ptB[pi]])
                pv_ = pt[:, 0:nt_ * 128].rearrange("p (t a) -> p t a", t=nt_)
                P.op("dve", lambda e, pv_=pv_, nt_=nt_, tidx=tidx: e.tensor_tensor(
                    out=pv_, in0=pv_,
                    in1=et[slot][:, hl * 5 + tidx, :].unsqueeze(1).to_broadcast([128, nt_, 128]),
                    op=ALU.mult), r=[etB[slot]], w=[ptB[pi]])

                def pv(e, g=g, half=half, tiles=tiles, pt=pt, nblk=nblk):
                    ins = None
                    for ti, (r_, n_) in enumerate(tiles):
                        kt = r_ * nblk + (n_ if half == 0 else n_ - 1)
                        lhsT = Vall[:, g, kt, 0:128] if hl == 0 else Vall[:, g, kt, 64:192]
                        if g == 0:
                            outs = [(4 + n_ // 4, slice((n_ % 4) * 128, (n_ % 4 + 1) * 128),
                                     slice(ti * 128, (ti + 1) * 128))]
                        elif g == 1:
                            outs = [(4 + n_, slice(r_, 512, 4), slice(ti * 128, (ti + 1) * 128))]
                        else:
                            outs = [(4 + b, slice(r_, 512, 16), slice(ti * 128 + 32 * b, ti * 128 + 32 * b + 32))
                                    for b in range(4)]
                        for (bk, osl, rsl) in outs:
                            first = bk not in started
                            started.add(bk)
                            ins = e.matmul(C.ps[bk][:, osl], lhsT, pt[:, rsl], start=first, stop=True,
                                           skip_group_check=True)
                    return ins
                if deferred and bi_ % 4 == 2:
                    deferred.pop(0)()
                pend.append((pv, [ptB[pi], vB[g]]))
                if len(pend) > 2:
                    f_, r_ = pend.pop(0)
                    P.op("pe", f_, r=r_, pw=C.psB[4:8])
            for f_, r_ in pend:
                P.op("pe", f_, r=r_, pw=C.psB[4:8])
            if hl == 0:
                num, den = slice(0, 64), slice(64, 128)
            else:
                num, den = slice(64, 128), slice(0, 64)
            for b in range(4):
                cs = slice(b * 512, (b + 1) * 512)
                P.op("act", lambda e, b=b, cs=cs: e.activation(out=VT[num, cs], in_=C.ps[4 + b][num, :],
                                                               func=AF.Copy, scale=gone[num, 0:1]),
                     r=[C.psB[4 + b], goneB], pw=[vtB])
                P.op("dve", lambda e, b=b, cs=cs: e.tensor_copy(out=rc[num, cs], in_=C.ps[4 + b][den, :]),
                     r=[C.psB[4 + b]], pw=[rcB])
            while deferred:
                deferred.pop(0)()
            mine = []
            for b in range(4):
                def nrm(b=b, num=num):
                    cs = slice(b * 512, (b + 1) * 512)
                    P.op("dve", lambda e: e.reciprocal(out=rc[num, cs], in_=rc[num, cs]),
                         r=[rcB], w=[rcB2[b]])
                    P.op("dve", lambda e: e.tensor_tensor(out=OT[num, cs], in0=VT[num, cs], in1=rc[num, cs],
                                                          op=ALU.mult),
                         r=[vtB, rcB, rcB2[b]], pw=[otB[b]])
                mine.append(nrm)
            return mine

        def out_proj(j, deferred=()):
            slot = j % 2
            deferred = list(deferred)
            for ti in range(NT):
                if ti % 4 == 0 and deferred:
                    deferred.pop(0)()
                for half in range(2):
                    bank = 2 + state["pr"] % 2
                    state["pr"] += 1
                    P.op("pe", lambda e, ti=ti, half=half, bank=bank: e.matmul(
                        C.ps[bank][:, :], OT[:, ti * 128:(ti + 1) * 128],
                        wo[slot][:, half * 512:(half + 1) * 512], start=True, stop=True),
                        r=[otB[ti // 4], woB[slot]], w=[C.psB[bank]])
                    hs = C.h[:, ti, half * 512:(half + 1) * 512]
                    P.op("dve", lambda e, hs=hs, bank=bank: e.tensor_tensor(
                        out=hs, in0=C.ps[bank][:, :], in1=hs, op=ALU.add),
                        r=[C.psB[bank]], w=[C.hB[ti]])

        npairs = getattr(C, "dbg_pairs", 8)
        if getattr(C, "dbg_stop", None) == "xt":
            return
        load_pair_inputs(0)
        load_pair_weights(0)
        if getattr(C, "dbg_stop", None) == "loads":
            return
        for j in range(npairs):
            project_pair()
            if getattr(C, "dbg_stop", None) == "proj":
                return
            if j + 1 < npairs:
                load_pair_inputs(j + 1)
            dA = attend_head(j, 0)
            dB = attend_head(j, 1, dA)
            if j + 1 < npairs:
                load_pair_weights(j + 1)
            out_proj(j, dB)

def phase_c(C, dram, out_ap):
    nc, P = C.nc, C.P
    with contextlib.ExitStack() as st:
        _const_begin(C, "d_constC")
        gF, gFB = _load_gain_cols(C, "gF1", dram["ffn_norm1"])
        fng, fngB = _load_bcast_row(C, st, "fng", dram["final_norm"])
        xh = st.enter_context(nc.sbuf_tensor("xhc", [128, NT, D], BF16))
        xhB = [Buf() for _ in range(NT)]
        XT = st.enter_context(nc.sbuf_tensor("XTc", [128, KD, S], BF16))
        XTB = [Buf() for _ in range(4)]
        wr = st.enter_context(nc.sbuf_tensor("wr", [128, KD, NEXP], BF16))
        wrB = Buf()
        P.dma("pool", C.d_const, wr[:], dram["moe_router"].rearrange("(k p) e -> p k e", p=128),
              w=[wrB])
        C.const_bufs.append(wrB)
        _const_fence(C)
        _rms_stats(C)
        _normalize_tm(C, xh, xhB)
        _transpose_gain(C, xh, xhB, XT, XTB, gF, gFB)
        L = st.enter_context(nc.sbuf_tensor("L", [128, NT, NEXP], F32))
        L2 = st.enter_context(nc.sbuf_tensor("L2", [128, NT, NEXP], F32))
        gate = st.enter_context(nc.sbuf_tensor("gate", [128, NT, NEXP], F32))
        m1 = st.enter_context(nc.sbuf_tensor("m1", [128, NT], F32))
        m2 = st.enter_context(nc.sbuf_tensor("m2", [128, NT], F32))
        LB, L2B, gateB, m1B, m2B = Buf(), Buf(), Buf(), Buf(), Buf()

        def rt(e):
            ins = None
            for i in range(NT):
                for k in range(KD):
                    ins = e.matmul(C.ps[0][:, i * NEXP:(i + 1) * NEXP], XT[:, k, i * 128:(i + 1) * 128],
                                   wr[:, k, :], start=(k == 0), stop=(k == KD - 1),
                                   skip_group_check=True)
            return ins
        P.op("pe", rt, r=XTB + [wrB], w=[C.psB[0]])
        psl = C.ps[0][:, 0:NT * NEXP].rearrange("p (i e) -> p i e", e=NEXP)
        P.op("dve", lambda e: e.tensor_copy(out=L[:], in_=psl), r=[C.psB[0]], w=[LB])
        P.op("dve", lambda e: e.tensor_reduce(out=m1[:], in_=L[:], axis=AX.X, op=ALU.max),
             r=[LB], w=[m1B])
        bc = lambda t: t[:, :].unsqueeze(2).to_broadcast([128, NT, NEXP])
        P.op("dve", lambda e: e.tensor_tensor(out=L2[:], in0=L[:], in1=bc(m1), op=ALU.is_equal),
             r=[LB, m1B], w=[L2B])
        P.op("dve", lambda e: e.scalar_tensor_tensor(out=L2[:], in0=L2[:], scalar=-1e30, in1=L[:],
                                                     op0=ALU.mult, op1=ALU.add),
             r=[LB], w=[L2B])
        P.op("dve", lambda e: e.tensor_reduce(out=m2[:], in_=L2[:], axis=AX.X, op=ALU.max),
             r=[L2B], w=[m2B])
        P.op("dve", lambda e: e.tensor_tensor(out=L2[:], in0=L[:], in1=bc(m2), op=ALU.is_ge),
             r=[LB, m2B], w=[L2B])
        P.op("dve", lambda e: e.tensor_tensor(out=gate[:], in0=L[:], in1=bc(m1), op=ALU.subtract),
             r=[LB, m1B], w=[gateB])
        P.op("act", lambda e: e.activation(out=gate[:], in_=gate[:], func=AF.Exp),
             r=[], w=[gateB])
        P.op("dve", lambda e: e.tensor_tensor(out=gate[:], in0=gate[:], in1=L2[:], op=ALU.mult),
             r=[L2B], w=[gateB])
        P.op("dve", lambda e: e.tensor_reduce(out=m1[:], in_=gate[:], axis=AX.X, op=ALU.add),
             r=[gateB], w=[m1B])
        P.op("dve", lambda e: e.reciprocal(out=m1[:], in_=m1[:]), r=[], w=[m1B])
        P.op("dve", lambda e: e.tensor_tensor(out=gate[:], in0=gate[:], in1=bc(m1), op=ALU.mult),
             r=[m1B], w=[gateB])
        _alloc_ffn(C, st, "C")
        for ex in range(NEXP):
            _ffn(C, st, XT, XTB, dram["moe_w_gate"][ex], dram["moe_w_up"][ex], dram["moe_w_down"][ex],
                 FF_EXP, "e%d" % ex, gate=gate, gateB=gateB, gate_e=ex)
        _rms_stats(C)
        ov = out_ap.rearrange("(n p) d -> p n d", p=128)
        for i in range(NT):
            P.op("dve", lambda e, i=i: e.scalar_tensor_tensor(
                out=C.h[:, i, :], in0=C.h[:, i, :], scalar=C.rstd[:, i:i + 1], in1=fng[:],
                op0=ALU.mult, op1=ALU.mult), r=[C.rstdB, fngB], w=[C.hB[i]])
            if i % 4 == 3:
                tok = P.dma("sp", C.d_io, ov[:, i - 3:i + 1, :], C.h[:, i - 3:i + 1, :],
                            r=C.hB[i - 3:i + 1])
                P.out_toks.append(tok)


def phase_c_sparse(C, dram, out_ap):
    nc, P = C.nc, C.P
    with contextlib.ExitStack() as st:
        _const_begin(C, "d_constC")
        gF, gFB = _load_gain_cols(C, "gF1", dram["ffn_norm1"])
        fng, fngB = _load_bcast_row(C, st, "fng", dram["final_norm"])
        wr = st.enter_context(nc.sbuf_tensor("wr", [128, KD, NEXP], BF16))
        wrB = Buf()
        P.dma("pool", C.d_const, wr[:], dram["moe_router"].rearrange("(k p) e -> p k e", p=128), w=[wrB])
        C.const_bufs.append(wrB)
        iota = st.enter_context(nc.sbuf_tensor("iota", [128, CAP], F32))
        iotaB = Buf()
        P.dma("sp", C.d_const, iota[:], dram["c_iota"], w=[iotaB])
        C.const_bufs.append(iotaB)
        tri = st.enter_context(nc.sbuf_tensor("tri", [128, 128], BF16))
        triB = Buf()
        P.dma("pool", C.d_const, tri[:], dram["c_tri"], w=[triB])
        C.const_bufs.append(triB)
        _const_fence(C)
        ones = st.enter_context(nc.sbuf_tensor("onesb", [128, 128], BF16))
        onesB = Buf()
        P.op("dve", lambda e: e.memset(ones[:], 1.0), w=[onesB])
        onec = st.enter_context(nc.sbuf_tensor("onec", [128, 1], F32))
        onecB = Buf()
        P.op("dve", lambda e: e.memset(onec[:], 1.0), w=[onecB])

        xh = st.enter_context(nc.sbuf_tensor("xhc", [128, NT, D], BF16))
        xhB = [Buf() for _ in range(NT)]
        L = st.enter_context(nc.sbuf_tensor("L", [128, NT, NEXP], F32))
        L2 = st.enter_context(nc.sbuf_tensor("L2", [128, NT, NEXP], F32))
        gate = st.enter_context(nc.sbuf_tensor("gate", [128, NT, NEXP], F32))
        rank = st.enter_context(nc.sbuf_tensor("rank", [128, NT, NEXP], F32))
        tot = st.enter_context(nc.sbuf_tensor("tot", [128, NT, NEXP], F32))
        off = st.enter_context(nc.sbuf_tensor("off", [128, NT, NEXP], F32))
        maskb = st.enter_context(nc.sbuf_tensor("maskb", [128, NT, NEXP], BF16))
        m1 = st.enter_context(nc.sbuf_tensor("m1", [128, NT], F32))
        m2 = st.enter_context(nc.sbuf_tensor("m2", [128, NT], F32))
        LB, L2B, gateB, m1B, m2B, rankB, totB, offB, maskbB = (Buf() for _ in range(9))

        with contextlib.ExitStack() as st2:
            XT = st2.enter_context(nc.sbuf_tensor("XTc", [128, KD, S], BF16))
            XTB = [Buf() for _ in range(4)]
            _rms_stats(C)
            _normalize_tm(C, xh, xhB)
            _transpose_gain(C, xh, xhB, XT, XTB, gF, gFB)

            def rt(e):
                ins = None
                for i in range(NT):
                    for k in range(KD):
                        ins = e.matmul(C.ps[0][:, i * NEXP:(i + 1) * NEXP], XT[:, k, i * 128:(i + 1) * 128],
                                       wr[:, k, :], start=(k == 0), stop=(k == KD - 1),
                                       skip_group_check=True)
                return ins
            P.op("pe", rt, r=XTB + [wrB], w=[C.psB[0]])
            psl = C.ps[0][:, 0:NT * NEXP].rearrange("p (i e) -> p i e", e=NEXP)
            P.op("dve", lambda e: e.tensor_copy(out=L[:], in_=psl), r=[C.psB[0]], w=[LB])
        P.barrier()

        bc = lambda t: t[:, :].unsqueeze(2).to_broadcast([128, NT, NEXP])
        P.op("dve", lambda e: e.tensor_reduce(out=m1[:], in_=L[:], axis=AX.X, op=ALU.max), r=[LB], w=[m1B])
        P.op("dve", lambda e: e.tensor_tensor(out=L2[:], in0=L[:], in1=bc(m1), op=ALU.is_equal),
             r=[LB, m1B], w=[L2B])
        P.op("dve", lambda e: e.scalar_tensor_tensor(out=L2[:], in0=L2[:], scalar=-1e30, in1=L[:],
                                                     op0=ALU.mult, op1=ALU.add), r=[LB], w=[L2B])
        P.op("dve", lambda e: e.tensor_reduce(out=m2[:], in_=L2[:], axis=AX.X, op=ALU.max), r=[L2B], w=[m2B])
        P.op("dve", lambda e: e.tensor_tensor(out=L2[:], in0=L[:], in1=bc(m2), op=ALU.is_ge),
             r=[LB, m2B], w=[L2B])
        P.op("dve", lambda e: e.tensor_tensor(out=gate[:], in0=L[:], in1=bc(m1), op=ALU.subtract),
             r=[LB, m1B], w=[gateB])
        P.op("act", lambda e: e.activation(out=gate[:], in_=gate[:], func=AF.Exp), r=[], w=[gateB])
        P.op("dve", lambda e: e.tensor_tensor(out=gate[:], in0=gate[:], in1=L2[:], op=ALU.mult),
             r=[L2B], w=[gateB])
        P.op("dve", lambda e: e.tensor_reduce(out=m1[:], in_=gate[:], axis=AX.X, op=ALU.add),
             r=[gateB], w=[m1B])
        P.op("dve", lambda e: e.reciprocal(out=m1[:], in_=m1[:]), r=[], w=[m1B])
        P.op("dve", lambda e: e.tensor_tensor(out=gate[:], in0=gate[:], in1=bc(m1), op=ALU.mult),
             r=[m1B], w=[gateB])
        P.op("dve", lambda e: e.tensor_copy(out=maskb[:], in_=L2[:]), r=[L2B], w=[maskbB])
        mflat = maskb[:, :, :].rearrange("p i e -> p (i e)")
        P.op("pe", lambda e: e.matmul(C.ps[0][:, 0:128], tri[:], mflat, start=True, stop=True),
             r=[triB, maskbB], w=[C.psB[0]])
        P.op("pe", lambda e: e.matmul(C.ps[1][:, 0:128], ones[:], mflat, start=True, stop=True),
             r=[onesB, maskbB], w=[C.psB[1]])
        v3 = lambda ps_: ps_[:, 0:128].rearrange("p (i e) -> p i e", e=NEXP)
        P.op("dve", lambda e: e.tensor_copy(out=rank[:], in_=v3(C.ps[0])), r=[C.psB[0]], w=[rankB])
        P.op("dve", lambda e: e.tensor_copy(out=tot[:], in_=v3(C.ps[1])), r=[C.psB[1]], w=[totB])
        P.op("dve", lambda e: e.memset(off[:], 0.0), w=[offB])
        for i in range(1, NT):
            P.op("dve", lambda e, i=i: e.tensor_tensor(out=off[:, i, :], in0=off[:, i - 1, :],
                                                       in1=tot[:, i - 1, :], op=ALU.add),
                 r=[totB], w=[offB])
        P.op("dve", lambda e: e.tensor_tensor(out=rank[:], in0=rank[:], in1=off[:], op=ALU.add),
             r=[offB], w=[rankB])

        SelT = st.enter_context(nc.sbuf_tensor("SelT", [128, NCT, S], BF16))
        selTB = Buf()
        selr = [st.enter_context(nc.sbuf_tensor("selr%d" % i, [128, CAP], BF16)) for i in range(4)]
        selB = [Buf() for _ in range(4)]
        XTe = st.enter_context(nc.sbuf_tensor("XTe", [128, KD, CAP], BF16))
        XTeB = [Buf(), Buf()]
        ye = st.enter_context(nc.sbuf_tensor("ye", [128, NCT, D], F32))
        yeB = [Buf() for _ in range(NCT)]
        yb = st.enter_context(nc.sbuf_tensor("ybf", [128, NCT, D], BF16))
        ybB = [Buf() for _ in range(NCT)]
        _alloc_ffn(C, st, "C", gw=256)
        chunks = [(0, 384), (384, 256)]
        rot = {"s": 0, "t": 0, "y": 0}

        for ex in range(NEXP):
            for kh in range(2):
                for i in range(NT):
                    si = rot["s"] % 4
                    rot["s"] += 1
                    sel = selr[si]
                    P.op("dve", lambda e, i=i, sel=sel, ex=ex: e.tensor_scalar(
                        out=sel[:], in0=iota[:], scalar1=rank[:, i, ex:ex + 1], scalar2=L2[:, i, ex:ex + 1],
                        op0=ALU.is_equal, op1=ALU.mult), r=[iotaB, rankB, L2B], w=[selB[si]])
                    if kh == 0:
                        tb = 5 + rot["t"] % 2
                        rot["t"] += 1
                        psb = C.ps[tb].bitcast(BF16)

                        def trs(e, sel=sel, psb=psb):
                            ins = None
                            for cc in range(NCT):
                                ins = e.transpose(psb[:, cc * 128:(cc + 1) * 128], sel[:, cc * 128:(cc + 1) * 128],
                                                  C.ident[:])
                            return ins
                        P.op("pe", trs, r=[selB[si], C.identB], w=[C.psB[tb]])
                        srcv = psb[:, 0:CAP].rearrange("p (c t) -> p c t", c=NCT)

                        def evs(e, i=i, srcv=srcv):
                            ins = None
                            for cc in range(NCT):
                                ins = e.activation(out=SelT[:, cc, i * 128:(i + 1) * 128], in_=srcv[:, cc, :],
                                                   func=AF.Copy, scale=onec[:, 0:1])
                            return ins
                        P.op("act", evs, r=[C.psB[tb], onecB], pw=[selTB])

                    def ga(e, i=i, kh=kh, sel=sel):
                        ins = None
                        for kk in range(4):
                            k = kh * 4 + kk
                            lt = xh[:, i, k * 128:(k + 1) * 128]
                            e.matmul(C.ps[kk][:, :], lt, sel[:, 0:512], start=(i == 0), stop=(i == NT - 1),
                                     skip_group_check=True)
                            ins = e.matmul(C.ps[4][:, kk * 128:(kk + 1) * 128], lt, sel[:, 512:CAP],
                                           start=(i == 0 and kk == 0), stop=(i == NT - 1), skip_group_check=True)
                        return ins
                    if i == 0:
                        P.op("pe", ga, r=[xhB[i], selB[si]], w=C.psB[0:5])
                    else:
                        P.op("pe", ga, r=[xhB[i], selB[si]], pw=C.psB[0:5])
                for kk in range(4):
                    k = kh * 4 + kk
                    P.op("act", lambda e, k=k, kk=kk: e.activation(
                        out=XTe[:, k, 0:512], in_=C.ps[kk][:, :], func=AF.Copy, scale=gF[:, k:k + 1]),
                        r=[C.psB[kk], gFB], pw=XTeB)
                    P.op("act", lambda e, k=k, kk=kk: e.activation(
                        out=XTe[:, k, 512:CAP], in_=C.ps[4][:, kk * 128:(kk + 1) * 128], func=AF.Copy,
                        scale=gF[:, k:k + 1]), r=[C.psB[4], gFB], pw=XTeB)
            for cc in range(NCT):
                P.op("pool", lambda e, cc=cc: e.memset(ye[:, cc, :], 0.0), w=[yeB[cc]])
            _ffn(C, st, XTe, XTeB, dram["moe_w_gate"][ex], dram["moe_w_up"][ex], dram["moe_w_down"][ex],
                 FF_EXP, "e%d" % ex, chunks=chunks, tgt=ye, tgtB=yeB)
            for cc in range(NCT):
                P.op("act", lambda e, cc=cc: e.activation(out=yb[:, cc, :], in_=ye[:, cc, :], func=AF.Copy,
                                                          scale=onec[:, 0:1]),
                     r=[yeB[cc], onecB], w=[ybB[cc]])
            for i in range(NT):
                for half in range(2):
                    bk = 4 + rot["y"] % 4
                    rot["y"] += 1

                    def sc(e, i=i, half=half, bk=bk):
                        ins = None
                        for cc in range(NCT):
                            ins = e.matmul(C.ps[bk][:, :], SelT[:, cc, i * 128:(i + 1) * 128],
                                           yb[:, cc, half * 512:(half + 1) * 512],
                                           start=(cc == 0), stop=(cc == NCT - 1))
                        return ins
                    P.op("pe", sc, r=[selTB] + ybB, w=[C.psB[bk]])
                    hs = C.h[:, i, half * 512:(half + 1) * 512]
                    gap = gate[:, i, ex:ex + 1]
                    P.op("dve", lambda e, bk=bk, hs=hs, gap=gap: e.scalar_tensor_tensor(
                        out=hs, in0=C.ps[bk][:, :], scalar=gap, in1=hs, op0=ALU.mult, op1=ALU.add),
                        r=[C.psB[bk], gateB], w=[C.hB[i]])

        _rms_stats(C)
        ov = out_ap.rearrange("(n p) d -> p n d", p=128)
        for i in range(NT):
            P.op("dve", lambda e, i=i: e.scalar_tensor_tensor(
                out=C.h[:, i, :], in0=C.h[:, i, :], scalar=C.rstd[:, i:i + 1], in1=fng[:],
                op0=ALU.mult, op1=ALU.mult), r=[C.rstdB, fngB], w=[C.hB[i]])
            if i % 4 == 3:
                tok = P.dma("sp", C.d_io, ov[:, i - 3:i + 1, :], C.h[:, i - 3:i + 1, :],
                            r=C.hB[i - 3:i + 1])
                P.out_toks.append(tok)

_SPECS = {
    "x": [S, D], "a_norm": [D], "a_proj": [4, 256, 256], "a_scale": [D], "kv_norm": [D],
    "w_kv": [D, 6144], "b_norm": [D], "w_q": [D, 3072], "w_o": [D, D], "rel_bias": [32, 48],
    "ffn_norm0": [D], "ffn_norm1": [D], "dense_w_gate": [D, FF_DENSE], "dense_w_up": [D, FF_DENSE],
    "dense_w_down": [FF_DENSE, D], "moe_router": [D, NEXP], "moe_w_gate": [NEXP, D, FF_EXP],
    "moe_w_up": [NEXP, D, FF_EXP], "moe_w_down": [NEXP, FF_EXP, D], "final_norm": [D],
    "c_ident": [128, 128], "c_band": [4, 3, 128, 128], "c_oh": [3, 64, 512], "c_iota": [128, CAP], "c_tri": [128, 128],
}


def build(phases, in_names, hin="x", dbg_stop=None):
    nc = bass.Bass("TRN2", target_bir_lowering=False)
    dram = {}
    for n in in_names:
        dram[n] = nc.dram_tensor(n, _SPECS.get(n, [S, D]), F32, kind="ExternalInput").ap()
    out = nc.dram_tensor("out", [S, D], F32, kind="ExternalOutput").ap()
    with contextlib.ExitStack() as st:
        P = Prog(nc, st)
        C = _alloc_common(nc, st, P)
        C.dbg_stop = dbg_stop
        _const_begin(C, "d_const0")
        _load_ident(C, dram)
        _const_fence(C)
        _load_h(C, dram[hin])
        last = phases[-1]
        for pi_, ph in enumerate(phases):
            if pi_ > 0:
                P.barrier()
            if ph == "a":
                phase_a(C, dram)
            elif ph == "b":
                phase_b(C, dram)
            elif ph == "c":
                phase_c_sparse(C, dram, out)
            elif ph == "cd":
                phase_c(C, dram, out)
        if last != "c":
            _store_h(C, out)
        P.finish()
    return nc


_IN_A = ["x", "a_norm", "a_proj", "a_scale", "ffn_norm0", "dense_w_gate", "dense_w_up",
         "dense_w_down", "c_ident", "c_band"]
_IN_B = ["hin", "kv_norm", "w_kv", "b_norm", "w_q", "w_o", "rel_bias", "c_ident", "c_oh"]
_IN_C = ["hin", "ffn_norm1", "moe_router", "moe_w_gate", "moe_w_up", "moe_w_down", "final_norm",
         "c_ident", "c_iota", "c_tri"]


def _prep(inputs):
    f = lambda a: np.ascontiguousarray(np.asarray(a, dtype=np.float32))
    d = {
        "a_norm": f(inputs["a_norm"][0]), "a_proj": f(inputs["a_proj"][0]),
        "a_scale": f(inputs["a_scale"][0]), "kv_norm": f(inputs["kv_norm"]),
        "w_kv": f(inputs["w_kv"]), "b_norm": f(inputs["b_norm"][0]), "w_q": f(inputs["w_q"][0]),
        "w_o": f(inputs["w_o"][0]), "rel_bias": f(inputs["rel_bias"]),
        "ffn_norm0": f(inputs["ffn_norm"][0]), "ffn_norm1": f(inputs["ffn_norm"][1]),
        "dense_w_gate": f(inputs["dense_w_gate"][0]), "dense_w_up": f(inputs["dense_w_up"][0]),
        "dense_w_down": f(inputs["dense_w_down"][0]), "moe_router": f(inputs["moe_router"][0]),
        "moe_w_gate": f(inputs["moe_w_gate"][0]), "moe_w_up": f(inputs["moe_w_up"][0]),
        "moe_w_down": f(inputs["moe_w_down"][0]), "final_norm": f(inputs["final_norm"]),
    }
    d.update(_host_consts())
    return d


def _run(nc, names, shared, per_core_key, per_core_vals):
    in_maps = []
    for c in range(NCORES):
        m = {n: shared[n] for n in names if n != per_core_key}
        m[per_core_key] = per_core_vals[c]
        in_maps.append(m)
    res = run_bass_kernel_spmd(nc, in_maps, core_ids=list(range(NCORES)))
    return [np.asarray(r["out"]) for r in res.results]


_IN_ALL = ["x", "a_norm", "a_proj", "a_scale", "kv_norm", "w_kv", "b_norm", "w_q", "w_o", "rel_bias",
           "ffn_norm0", "ffn_norm1", "dense_w_gate", "dense_w_up", "dense_w_down", "moe_router",
           "moe_w_gate", "moe_w_up", "moe_w_down", "final_norm", "c_ident", "c_band", "c_oh", "c_iota",
           "c_tri"]


def kernel(**inputs):
    shared = _prep(inputs)
    x = np.ascontiguousarray(np.asarray(inputs["x"], dtype=np.float32))
    xs = [x[b] for b in range(NCORES)]
    nc = build(["a", "b", "c"], _IN_ALL, hin="x")
    h = _run(nc, _IN_ALL, shared, "x", xs)
    return np.stack(h, axis=0).astype(np.float32)
```
